# Optimizing a Trainium2 kernel written in Bass

```python
import jax, jax.numpy as jnp
from jax import lax
import numpy as np

D_MODEL = 1024
BATCH = 8
SEQ = 4096
DEPTH = 1

D_MIX = D_MODEL
N_HEADS = 8
HEAD_DIM = 64
N_KV_HEADS = 2
Q_PER_KV = N_HEADS // N_KV_HEADS
D_ATTN = N_HEADS * HEAD_DIM
IDX_HEADS = 8
IDX_DIM = 64
TOPK_MAX = 256
Q_BLOCK = 128
POOL_WINDOWS = (2, 4, 8, 16)
N_POOL_GROUPS = len(POOL_WINDOWS)
D_POOL = D_MIX - D_ATTN
POOL_GROUP_DIM = D_POOL // N_POOL_GROUPS

Q_COLS = N_HEADS * HEAD_DIM
KV_COLS = N_KV_HEADS * HEAD_DIM
QI_COLS = IDX_HEADS * IDX_DIM
KI_COLS = IDX_DIM
WI_COLS = IDX_HEADS
SPLIT_POINTS = (Q_COLS, Q_COLS + KV_COLS, Q_COLS + 2 * KV_COLS, Q_COLS + 2 * KV_COLS + D_POOL,
                Q_COLS + 2 * KV_COLS + D_POOL + QI_COLS, Q_COLS + 2 * KV_COLS + D_POOL + QI_COLS + KI_COLS)
IN_COLS = Q_COLS + 2 * KV_COLS + D_POOL + QI_COLS + KI_COLS + WI_COLS

N_EXPERT_GROUPS = 4
EXPERTS_PER_GROUP = 4
TOP_K_INNER = 2
D_EXPERT = 256

ALPHA = float((2 * DEPTH) ** 0.25)
BETA = float((8 * DEPTH) ** -0.25)
LN_EPS = 1e-5

kernel_name = "hymba_dsa_pool_hmoe_deepnorm"


def _layer_norm(z, g, b):
    z32 = z.astype(jnp.float32)
    mu = jnp.mean(z32, axis=-1, keepdims=True)
    var = jnp.mean(jnp.square(z32 - mu), axis=-1, keepdims=True)
    y = (z32 - mu) * lax.rsqrt(var + LN_EPS) * g.astype(jnp.float32) + b.astype(jnp.float32)
    return y.astype(z.dtype)


def _dsa_attention(q, k, v, q_idx, k_idx, w_idx):
    B, S = q.shape[0], q.shape[1]
    topk = min(TOPK_MAX, S // 4)
    n_blk = S // Q_BLOCK
    key_pos = jnp.arange(S)
    idx_scale = (IDX_DIM ** -0.5) * (IDX_HEADS ** -0.5)
    attn_scale = HEAD_DIM ** -0.5
    k_idx32 = k_idx.astype(jnp.float32)

    def to_blocks(a):
        return jnp.swapaxes(a.reshape((B, n_blk, Q_BLOCK) + a.shape[2:]), 0, 1)

    def block_fn(args):
        qb, qib, wib, start = args
        q_pos = start + jnp.arange(Q_BLOCK)
        causal = key_pos[None, :] <= q_pos[:, None]
        logits = jnp.einsum('bqhd,bsd->bqhs', qib.astype(jnp.float32), k_idx32)
        score = jnp.einsum('bqhs,bqh->bqs', jax.nn.relu(logits), wib.astype(jnp.float32)) * idx_scale
        score = jnp.where(causal[None], score, -jnp.inf)
        _, sel = lax.top_k(score, topk)
        valid = sel <= q_pos[None, :, None]
        k_sel = jax.vmap(lambda kb, ib: kb[ib])(k, sel)
        v_sel = jax.vmap(lambda vb, ib: vb[ib])(v, sel)
        qg = qb.reshape(B, Q_BLOCK, N_KV_HEADS, Q_PER_KV, HEAD_DIM)
        s = jnp.einsum('bqkgd,bqnkd->bqkgn', qg, k_sel).astype(jnp.float32) * attn_scale
        s = jnp.where(valid[:, :, None, None, :], s, -jnp.inf)
        p = jax.nn.softmax(s, axis=-1)
        o = jnp.einsum('bqkgn,bqnkd->bqkgd', p.astype(v.dtype), v_sel)
        return o.reshape(B, Q_BLOCK, N_HEADS * HEAD_DIM)

    starts = jnp.arange(n_blk) * Q_BLOCK
    out = lax.map(block_fn, (to_blocks(q), to_blocks(q_idx), to_blocks(w_idx), starts))
    return jnp.swapaxes(out, 0, 1).reshape(B, S, N_HEADS * HEAD_DIM)


def _multiscale_pool(u, w_pool, pool_scale):
    B, S = u.shape[0], u.shape[1]
    u32 = u.astype(jnp.float32).reshape(B, S, N_POOL_GROUPS, POOL_GROUP_DIM)
    pos = jnp.arange(S)
    outs = []
    for g, win in enumerate(POOL_WINDOWS):
        ug = u32[:, :, g]
        c = jnp.cumsum(ug, axis=1)
        c_lag = jnp.pad(c, ((0, 0), (win, 0), (0, 0)))[:, :S]
        count = jnp.minimum(pos + 1, win).astype(jnp.float32)[None, :, None]
        outs.append((c - c_lag) / count - ug)
    d = jnp.stack(outs, axis=2)
    y = jnp.einsum('bsgc,gcd->bsgd', d, w_pool.astype(jnp.float32)).reshape(B, S, D_POOL)
    return (y * pool_scale.astype(jnp.float32)).astype(u.dtype)


def _hier_moe(h, w_group_router, b_group_router, w_expert_router, b_expert_router, w_gate, w_up, w_down):
    B, S, D = h.shape
    t = h.reshape(B * S, D)
    t32 = t.astype(jnp.float32)
    g_logits = t32 @ w_group_router.astype(jnp.float32) + b_group_router.astype(jnp.float32)
    g_prob = jax.nn.softmax(g_logits, axis=-1)
    g_sel = jnp.argmax(g_logits, axis=-1)
    g_weight = jnp.take_along_axis(g_prob, g_sel[:, None], axis=-1)
    e_logits = (t32 @ w_expert_router.astype(jnp.float32) + b_expert_router.astype(jnp.float32))
    e_logits = e_logits.reshape(-1, N_EXPERT_GROUPS, EXPERTS_PER_GROUP)
    e_sel_logits = jnp.take_along_axis(e_logits, g_sel[:, None, None], axis=1)[:, 0]
    top_val, top_idx = lax.top_k(e_sel_logits, TOP_K_INNER)
    top_w = jax.nn.softmax(top_val, axis=-1) * g_weight
    inner_gate = jnp.sum(jax.nn.one_hot(top_idx, EXPERTS_PER_GROUP, dtype=jnp.float32) * top_w[..., None], axis=1)
    gate = jax.nn.one_hot(g_sel, N_EXPERT_GROUPS, dtype=jnp.float32)[:, :, None] * inner_gate[:, None, :]
    y = jnp.zeros((B * S, D), jnp.float32)
    for g in range(N_EXPERT_GROUPS):
        a = jnp.einsum('td,edf->tef', t, w_gate[g])
        b = jnp.einsum('td,edf->tef', t, w_up[g])
        hid = jax.nn.silu(a) * b * gate[:, g, :, None].astype(t.dtype)
        y = y + jnp.einsum('tef,efd->td', hid, w_down[g]).astype(jnp.float32)
    return y.reshape(B, S, D).astype(h.dtype)


def setup_inputs(seed: int = 0) -> dict:
    key = jax.random.key(seed)
    ks = jax.random.split(key, 16)
    f32 = jnp.float32
    nrm = jax.random.normal
    L = DEPTH
    x = nrm(ks[0], (BATCH, SEQ, D_MODEL), f32)
    col_scale = jnp.concatenate([
        jnp.ones((Q_COLS + KV_COLS,), f32),
        jnp.full((KV_COLS + D_POOL,), BETA, f32),
        jnp.ones((QI_COLS + KI_COLS + WI_COLS,), f32)])
    w_in = nrm(ks[1], (L, D_MODEL, IN_COLS), f32) * (D_MODEL ** -0.5) * col_scale
    w_pool = nrm(ks[2], (L, N_POOL_GROUPS, POOL_GROUP_DIM, POOL_GROUP_DIM), f32) * (POOL_GROUP_DIM ** -0.5)
    pool_scale = 1.0 + 0.02 * nrm(ks[3], (L, D_POOL), f32)
    w_out = nrm(ks[4], (L, D_MIX, D_MODEL), f32) * (D_MIX ** -0.5) * BETA
    ln1_g = 1.0 + 0.02 * nrm(ks[5], (L, D_MODEL), f32)
    ln1_b = 0.02 * nrm(ks[6], (L, D_MODEL), f32)
    w_group_router = nrm(ks[7], (L, D_MODEL, N_EXPERT_GROUPS), f32) * (D_MODEL ** -0.5)
    b_group_router = 0.01 * nrm(ks[8], (L, N_EXPERT_GROUPS), f32)
    w_expert_router = nrm(ks[9], (L, D_MODEL, N_EXPERT_GROUPS * EXPERTS_PER_GROUP), f32) * (D_MODEL ** -0.5)
    b_expert_router = 0.01 * nrm(ks[10], (L, N_EXPERT_GROUPS * EXPERTS_PER_GROUP), f32)
    w_gate = nrm(ks[11], (L, N_EXPERT_GROUPS, EXPERTS_PER_GROUP, D_MODEL, D_EXPERT), f32) * (D_MODEL ** -0.5)
    w_up = nrm(ks[12], (L, N_EXPERT_GROUPS, EXPERTS_PER_GROUP, D_MODEL, D_EXPERT), f32) * (D_MODEL ** -0.5)
    w_down = nrm(ks[13], (L, N_EXPERT_GROUPS, EXPERTS_PER_GROUP, D_EXPERT, D_MODEL), f32) * (D_EXPERT ** -0.5) * BETA
    ln2_g = 1.0 + 0.02 * nrm(ks[14], (L, D_MODEL), f32)
    ln2_b = 0.02 * nrm(ks[15], (L, D_MODEL), f32)
    return {"x": x, "w_in": w_in, "w_pool": w_pool, "pool_scale": pool_scale, "w_out": w_out,
            "ln1_g": ln1_g, "ln1_b": ln1_b, "w_group_router": w_group_router,
            "b_group_router": b_group_router, "w_expert_router": w_expert_router,
            "b_expert_router": b_expert_router, "w_gate": w_gate, "w_up": w_up, "w_down": w_down,
            "ln2_g": ln2_g, "ln2_b": ln2_b}


def reference(x, w_in, w_pool, pool_scale, w_out, ln1_g, ln1_b, w_group_router, b_group_router,
              w_expert_router, b_expert_router, w_gate, w_up, w_down, ln2_g, ln2_b):
    B, S, _ = x.shape
    for l in range(DEPTH):
        proj = jnp.einsum('bsd,dc->bsc', x, w_in[l])
        q, k, v, pool_in, q_idx, k_idx, w_idx = jnp.split(proj, SPLIT_POINTS, axis=-1)
        q = q.reshape(B, S, N_HEADS, HEAD_DIM)
        k = k.reshape(B, S, N_KV_HEADS, HEAD_DIM)
        v = v.reshape(B, S, N_KV_HEADS, HEAD_DIM)
        q_idx = q_idx.reshape(B, S, IDX_HEADS, IDX_DIM)
        attn_out = _dsa_attention(q, k, v, q_idx, k_idx, w_idx)
        pool_out = _multiscale_pool(pool_in, w_pool[l], pool_scale[l])
        mix = jnp.einsum('bsc,cd->bsd', jnp.concatenate([attn_out, pool_out], axis=-1), w_out[l])
        x = _layer_norm(ALPHA * x + mix, ln1_g[l], ln1_b[l])
        ffn = _hier_moe(x, w_group_router[l], b_group_router[l], w_expert_router[l], b_expert_router[l],
                        w_gate[l], w_up[l], w_down[l])
        x = _layer_norm(ALPHA * x + ffn, ln2_g[l], ln2_b[l])
    return x
```

```python
import numpy as np
from contextlib import ExitStack
import concourse.bass as bass
import concourse.mybir as mybir
from concourse.bass_utils import run_bass_kernel_spmd

F32 = mybir.dt.float32
BF16 = mybir.dt.bfloat16
I32 = mybir.dt.int32
MBDT = mybir.dt.bfloat16
AF = mybir.ActivationFunctionType
ALU = mybir.AluOpType
AX = mybir.AxisListType

S = 4096
D = 1024
NFM = 14
NC_FM = NFM * 128
NC_TM = 136
NCOLS = NC_FM + NC_TM
N_BISECT = 16
ALPHA = float(2.0 ** 0.25)
LN_EPS = 1e-5
TOPK = 256
NEG_MASK = -30000.0
N_EXP = 16
POOL_WINDOWS = (2, 4, 8, 16)


class Prog:
    ENGS = ("sync", "act", "dve", "pool", "pe")

    def __init__(self, nc, stack):
        self.nc = nc
        self.stack = stack
        self.q = {e: [] for e in self.ENGS}
        self.cnt = {e: 0 for e in self.ENGS}
        self.seen = {e: {} for e in self.ENGS}
        self.esem = {e: stack.enter_context(nc.semaphore("sem_" + e)) for e in self.ENGS}
        self.dsem = {}
        self.dcnt = {}
        self.lastw = {}
        self.readers = {}

    def _deps(self, r, w):
        deps = []
        for b in r:
            t = self.lastw.get(b)
            if t is not None:
                deps.append(t)
        for b in w:
            t = self.lastw.get(b)
            if t is not None:
                deps.append(t)
            deps.extend(self.readers.get(b, ()))
        return deps

    def _commit(self, tok, r, w):
        for b in r:
            self.readers.setdefault(b, []).append(tok)
        for b in w:
            self.lastw[b] = tok
            self.readers[b] = []

    def _waits(self, eng, deps):
        need = {}
        for t in deps:
            if t[0] == "e":
                if t[1] == eng and eng == "pe":
                    continue
                key = ("e", t[1]); sem = self.esem[t[1]]; val = t[2]
            else:
                key = ("d", t[1]); sem = self.dsem[t[1]]; val = t[2]
            if key not in need or need[key][1] < val:
                need[key] = (sem, val)
        out = []
        for key, (sem, val) in need.items():
            if self.seen[eng].get(key, 0) >= val:
                continue
            self.seen[eng][key] = val
            out.append((sem, val))
        return out

    def op(self, eng, name, *args, r=(), w=(), **kw):
        waits = self._waits(eng, self._deps(r, w))
        self.cnt[eng] += 1
        tok = ("e", eng, self.cnt[eng])
        self.q[eng].append((waits, (name, args, kw), tok))
        self._commit(tok, r, w)
        return tok

    def dma(self, eng, key, out, in_, r=(), w=(), **kw):
        if key not in self.dsem:
            self.dsem[key] = self.stack.enter_context(self.nc.semaphore("dsem_" + key))
            self.dcnt[key] = 0
        waits = self._waits(eng, self._deps(r, w))
        self.dcnt[key] += 16
        tok = ("d", key, self.dcnt[key])
        kw = dict(kw); kw["out"] = out; kw["in_"] = in_
        self.q[eng].append((waits, ("dma_start", (), kw), tok))
        self._commit(tok, r, w)
        return tok

    def wait_all(self, eng, toks):
        waits = self._waits(eng, toks)
        self.q[eng].append((waits, None, None))

    def replay(self, eng, e):
        for waits, fn, tok in self.q[eng]:
            for sem, val in waits:
                e.wait_ge(sem, val)
            if fn is None:
                continue
            name, args, kw = fn
            ins = getattr(e, name)(*args, **kw)
            if tok[0] == "e":
                ins.then_inc(self.esem[eng], 1)
            else:
                ins.then_inc(self.dsem[tok[1]], 16)


def build_nc(npairs=16, n_bisect=N_BISECT):
    nc = bass.Bass("TRN2", target_bir_lowering=False)
    stack = ExitStack()
    P = Prog(nc, stack)

    def dram(name, shape, dt, kind):
        return nc.dram_tensor(name, list(shape), dt, kind=kind).ap()

    def sb(name, shape, dt):
        return stack.enter_context(nc.sbuf_tensor(name, list(shape), dt))

    x_d = dram("x", [S, D], F32, "ExternalInput")
    xT_d = dram("xT", [D, S], F32, "ExternalInput")
    win_d = dram("w_in_p", [D, NCOLS], F32, "ExternalInput")
    wpool_d = dram("w_pool", [4, 128, 128], F32, "ExternalInput")
    psc_d = dram("pool_scale", [512], F32, "ExternalInput")
    wout_d = dram("w_out", [D, D], F32, "ExternalInput")
    lnp_d = dram("ln_params", [4, D], F32, "ExternalInput")
    wr_d = dram("w_router", [D, 20], F32, "ExternalInput")
    br_d = dram("b_router", [20], F32, "ExternalInput")
    wgu_d = dram("w_gu", [N_EXP, D, 512], F32, "ExternalInput")
    wd_d = dram("w_d", [N_EXP, 256, D], F32, "ExternalInput")
    out_d = dram("out", [S, D], F32, "ExternalOutput")
    wgu_bf = dram("w_gu_bf", [N_EXP, D, 512], BF16, "Internal")
    wd_bf = dram("w_d_bf", [N_EXP, 256, D], BF16, "Internal")
    wfm_bf = dram("w_fm_bf", [NFM, 128, 8, 128], BF16, "Internal")
    wtm_bf = dram("w_tm_bf", [128, 8, NC_TM], BF16, "Internal")

    Wb = [sb("Wb%d" % i, [128, 8, 128], BF16) for i in range(4)]
    Wtm = sb("Wtm", [128, 8, NC_TM], BF16)
    Wout = sb("Wout", [128, 8, D], BF16)
    wpool = sb("wpool", [128, 4, 128], BF16)
    psc = sb("psc", [128, 4], F32)
    lnp = sb("lnp", [128, 4, D], F32)
    wr = sb("wr", [128, 8, 20], F32)
    br = sb("br", [128, 20], F32)
    iot_i = sb("iot_i", [128, 128], I32)
    iot_f = sb("iot_f", [128, 128], F32)
    ident32 = sb("ident32", [128, 128], F32)
    I4 = sb("I4", [128, 4, 128], BF16)
    cbias = sb("cbias", [128, 128], F32)
    iop1 = sb("iop1", [128, 1], F32)
    mrc = sb("mrc", [128, 4, 16], F32)
    negh = sb("negh", [128, 1], F32)
    CV = sb("CV", [128, 32], F32)
    WDS = sb("WDS", [128, 32], F32)
    NWDS = sb("NWDS", [128, 32], F32)
    kTz = sb("kTz", [128, 2, S], BF16)
    kiT = sb("kiT", [128, S], BF16)
    Vx = sb("Vx", [128, 32, 2, 65], BF16)
    wI = sb("wI", [128, 32, 8], F32)
    xTt = [sb("xTt%d" % i, [128, 8, 256], BF16) for i in range(2)]
    qTt = sb("qTt", [128, 4, 256], BF16)
    qiTz = sb("qiTz", [128, 8, 256], BF16)
    ug = [sb("ug%d" % g, [128, 272], F32) for g in range(4)]
    pa = sb("pa", [128, 272], F32)
    pb = sb("pb", [128, 272], F32)
    dTg = [sb("dTg%d" % i, [128, 256], BF16) for i in range(2)]
    poolTt = sb("poolTt", [128, 4, 256], BF16)
    SC = [sb("scores%d" % i, [128, S], F32) for i in range(2)]
    MBs = [sb("MB%d" % i, [128, S], MBDT) for i in range(2)]
    Rb = [sb("R%d" % i, [128, 512], F32) for i in range(3)]
    PT = [sb("PT%d" % i, [128, 512], BF16) for i in range(3)]
    Otok = sb("Otok", [128, 8, 64], BF16)
    OTs = sb("OTs", [128, 2, 512], F32)
    catT = sb("catT", [128, 4, 128], BF16)
    z = sb("z", [128, 2, D], F32)
    hTb = sb("hTb", [128, 8, 256], BF16)
    gate = sb("gate", [128, 2, 16], F32)
    gu = [sb("gu%d" % i, [128, 8, 512], BF16) for i in range(2)]
    dd = [sb("dd%d" % i, [128, 2, D], BF16) for i in range(2)]
    sa = [sb("sa%d" % i, [128, 256], F32) for i in range(2)]
    hid = [sb("hid%d" % i, [128, 256], BF16) for i in range(2)]
    hidT = [sb("hidT%d" % i, [128, 2, 128], BF16) for i in range(2)]
    sm = sb("sm", [128, 32], F32)
    st6 = sb("st6", [128, 2, 6], F32)
    st6b = sb("st6b", [128, 1, 12], F32)
    rt = sb("rt", [128, 92], F32)
    gu1f = gu[1][:].rearrange("p k f -> p (k f)").bitcast(F32)
    OTsb = gu1f[:, 0:1024].rearrange("p (g c) -> p g c", g=2)
    hT32 = gu1f[:, 1024:2048].rearrange("p (k t) -> p k t", k=8)

    ps = [stack.enter_context(nc.psum_tensor("ps%d" % i, [128, 512], F32)) for i in range(8)]

    def psb(i):
        return ps[i].bitcast(BF16) if hasattr(ps[i], "bitcast") else None

    MX, MN, LO, WD, MID, CNT, GE, KT, RSTD, NMR, TMP, NMID, CNT2, KT2, BB, NMID2, GE2 = range(17)

    def smc(i):
        return sm[:, i:i + 1]

    op = P.op
    op("pool", "iota", iot_i[:], w=["iot_i"], pattern=[[1, 128]], base=0, channel_multiplier=-1)
    op("pool", "tensor_copy", r=["iot_i"], w=["iot_f"], out=iot_f[:], in_=iot_i[:])
    op("pool", "tensor_scalar", r=["iot_f"], w=["ident32"], out=ident32[:], in0=iot_f[:], scalar1=0.0,
       scalar2=None, op0=ALU.is_equal)
    for j in range(4):
        op("pool", "tensor_copy", r=["ident32"], w=["I4"], out=I4[:, j, :], in_=ident32[:])
    op("pool", "tensor_scalar", r=["iot_f"], w=["cbias"], out=cbias[:], in0=iot_f[:], scalar1=0.0,
       scalar2=-1e30, op0=ALU.is_gt, op1=ALU.mult)
    op("pool", "tensor_scalar", r=["iot_f"], w=["iop1"], out=iop1[:], in0=iot_f[:, 0:1], scalar1=-1.0,
       scalar2=1.0, op0=ALU.mult, op1=ALU.add)
    op("pool", "tensor_scalar", r=["iot_f", "iop1"], w=["pa"], out=pa[:, 0:16], in0=iot_f[:, 0:16],
       scalar1=iop1[:, 0:1], scalar2=None, op0=ALU.add)
    for g, win in enumerate(POOL_WINDOWS):
        op("dve", "tensor_scalar", r=["pa"], w=["pb"], out=pb[:, 0:16], in0=pa[:, 0:16], scalar1=float(win),
           scalar2=1.0 / win, op0=ALU.min, op1=ALU.mult)
        op("dve", "reciprocal", r=["pb"], w=["mrc"], out=mrc[:, g, :], in_=pb[:, 0:16])
    op("pool", "memset", negh[:], -0.5, w=["negh"])
    for j in range(n_bisect + 1):
        op("pool", "memset", CV[:, j:j + 1], 0.5 ** j, w=["CV"])
    op("pool", "memset", kTz[:], 0.0, w=["kTz"])
    op("pool", "memset", qiTz[:], 0.0, w=["qiTz"])
    op("pool", "memset", Vx[:], 1.0, w=["Vx"])
    for g in range(4):
        op("pool", "memset", ug[g][:], 0.0, w=["ug%d" % g])

    stg = [SC[0][:, 0:2048], SC[0][:, 2048:4096]]
    tmpb = [MBs[0][:, 0:2048], MBs[0][:, 2048:4096]]
    wfm_v = wfm_bf.rearrange("b p k c -> p b k c")
    last = []
    for kc in range(8):
        sl = kc % 2
        P.dma("sync", "stg%d" % sl, stg[sl][:, 0:NCOLS], win_d[kc * 128:(kc + 1) * 128, :], w=["stg%d" % sl])
        if kc % 2 == 0:
            op("act", "activation", r=["stg%d" % sl], w=["tmpb%d" % sl], out=tmpb[sl][:, 0:NCOLS],
               in_=stg[sl][:, 0:NCOLS], func=AF.Copy)
        else:
            op("dve", "tensor_copy", r=["stg%d" % sl], w=["tmpb%d" % sl], out=tmpb[sl][:, 0:NCOLS],
               in_=stg[sl][:, 0:NCOLS])
        P.dma("sync", "wst%d" % sl, wfm_v[:, :, kc, :], tmpb[sl][:, 0:NC_FM].rearrange("p (b c) -> p b c", c=128),
              r=["tmpb%d" % sl], w=["winbf%d" % sl])
        P.dma("sync", "wst%d" % sl, wtm_bf[:, kc, :], tmpb[sl][:, NC_FM:NCOLS], r=["tmpb%d" % sl],
              w=["winbf%d" % sl])
    for kc in range(8):
        sl = kc % 2
        P.dma("sync", "stg%d" % sl, stg[sl][:, 0:D], wout_d[kc * 128:(kc + 1) * 128, :], w=["stg%d" % sl])
        if kc % 2 == 0:
            tk = op("act", "activation", r=["stg%d" % sl], w=["Wout"], out=Wout[:, kc, :], in_=stg[sl][:, 0:D],
                    func=AF.Copy)
        else:
            tk = op("dve", "tensor_copy", r=["stg%d" % sl], w=["Wout"], out=Wout[:, kc, :], in_=stg[sl][:, 0:D])
        last.append(tk)
    P.readers.setdefault("sc0", []).extend(last[-2:])
    for sl in range(2):
        P.readers.setdefault("MB0", []).append(P.lastw["winbf%d" % sl])
        P.readers.setdefault("MBh0", []).append(P.lastw["winbf%d" % sl])
    P.dma("sync", "wtm", Wtm[:], wtm_bf, r=["winbf0", "winbf1"], w=["Wtm"])
    for g in range(4):
        P.dma("pool", "setup_p", wpool[:, g, :], wpool_d[g], w=["wpool"])
        P.dma("sync", "setup_s", psc[:, g:g + 1], psc_d[g * 128:(g + 1) * 128].unsqueeze(1), w=["psc"])
    for i in range(4):
        P.dma("sync", "setup_s", lnp[:, i, :], lnp_d[i].partition_broadcast(128), w=["lnp"])
    P.dma("sync", "setup_s", wr[:], wr_d.rearrange("(k p) c -> p k c", p=128), w=["wr"])
    tok_setup = P.dma("sync", "setup_s", br[:], br_d.partition_broadcast(128), w=["br"])
    for nme in ("psc", "lnp", "wr", "br"):
        P.lastw[nme] = tok_setup
    P.lastw["wpool"] = ("d", "setup_p", P.dcnt["setup_p"])
    for g, win in enumerate(POOL_WINDOWS):
        op("dve", "tensor_scalar", r=["psc"], w=["psc"], out=psc[:, g:g + 1], in0=psc[:, g:g + 1],
           scalar1=1.0 / win, scalar2=None, op0=ALU.mult)

    xT_v = xT_d.rearrange("(k p) t -> p k t", p=128)

    def load_xT(pr_):
        nm = "xTt%d" % (pr_ % 2)
        P.dma("pool", nm, xTt[pr_ % 2][:], xT_v[:, :, pr_ * 256:pr_ * 256 + 256], w=[nm])

    load_xT(0)
    if npairs > 1:
        load_xT(1)
    def wcast(ex):
        for hlf in range(2):
            P.dma("pool", "wc%d" % ex, wgu_bf[ex, hlf * 512:(hlf + 1) * 512, :],
                  wgu_d[ex, hlf * 512:(hlf + 1) * 512, :], w=["wbf%d" % ex])
        P.dma("pool", "wc%d" % ex, wd_bf[ex], wd_d[ex], w=["wbf%d" % ex])
        P.lastw["wbf%d" % ex] = ("d", "wc%d" % ex, P.dcnt["wc%d" % ex])

    for ex in range(4):
        wcast(ex)

    def load_gu(ex):
        s_ = ex % 2
        P.dma("sync", "gu%d" % s_, gu[s_][:], wgu_bf[ex].rearrange("(k p) f -> p k f", p=128),
              r=["wbf%d" % ex], w=["gu%d" % s_])

    def load_dd(ex):
        s_ = ex % 2
        P.dma("sync", "dd%d" % s_, dd[s_][:], wd_bf[ex].rearrange("(k p) d -> p k d", p=128),
              r=["wbf%d" % ex], w=["dd%d" % s_])

    def layer_norm(zt, zn, gi, bi):
        for dh in range(2):
            op("dve", "bn_stats", r=[zn], w=["st6"], out=st6[:, dh, :], in_=zt[:, dh * 512:(dh + 1) * 512])
        op("dve", "bn_aggr", r=["st6"], w=["sm"], out=sm[:, 24:26], in_=st6[:].rearrange("p a b -> p (a b)"))
        op("dve", "tensor_scalar", r=["sm"], w=["sm"], out=smc(TMP), in0=sm[:, 25:26], scalar1=LN_EPS,
           scalar2=None, op0=ALU.add)
        op("pool", "tensor_tensor", r=["sm", "negh"], w=["sm2"], out=smc(RSTD), in0=smc(TMP), in1=negh[:],
           op=ALU.pow)
        op("dve", "scalar_tensor_tensor", r=["sm", "sm2"], w=["sm3"], out=smc(NMR), in0=sm[:, 24:25], scalar=-1.0,
           in1=smc(RSTD), op0=ALU.mult, op1=ALU.mult)
        op("act", "activation", r=[zn, "sm2", "sm3"], w=[zn], out=zt, in_=zt, func=AF.Identity, bias=smc(NMR),
           scale=smc(RSTD))
        op("dve", "tensor_tensor", r=[zn, "lnp"], w=[zn], out=zt, in0=zt, in1=lnp[:, gi, :], op=ALU.mult)
        op("pool", "tensor_tensor", r=[zn, "lnp"], w=[zn], out=zt, in0=zt, in1=lnp[:, bi, :], op=ALU.add)

    wb_ctr = [0]

    def proj_gen(pr, cbs, tokmajor):
        t0 = pr * 256
        xt = xTt[pr % 2]
        xn = "xTt%d" % (pr % 2)
        bi = 0
        slots = []

        def issue(cb_):
            wi_ = wb_ctr[0] % 4
            wb_ctr[0] += 1
            P.dma("sync", "Wb%d" % wi_, Wb[wi_][:], wfm_bf[cb_], r=["winbf0", "winbf1"], w=["Wb%d" % wi_])
            slots.append(wi_)

        for cb_ in cbs[:3]:
            issue(cb_)
        for ci_, cb in enumerate(cbs):
            if ci_ + 3 < len(cbs):
                issue(cbs[ci_ + 3])
            wi_ = slots[ci_]
            wn = "Wb%d" % wi_
            bank = bi % 2
            bi += 1
            bn = "ps%d" % bank
            pz = ps[bank]
            for kc in range(8):
                op("pe", "matmul", pz[:, 0:256], r=[wn, xn], w=[bn], lhsT=Wb[wi_][:, kc, :],
                   rhs=xt[:, kc, :], start=(kc == 0), stop=(kc == 7))
            if cb < 4:
                op("act", "activation", r=[bn], w=["qTt"], out=qTt[:, cb, :], in_=pz[:, 0:256], func=AF.Copy)
            elif cb == 4:
                op("act", "activation", r=[bn], w=["kTz"], out=kTz[0:64, 0, t0:t0 + 256], in_=pz[0:64, 0:256],
                   func=AF.Copy)
                op("act", "activation", r=[bn], w=["kTz"], out=kTz[64:128, 1, t0:t0 + 256], in_=pz[64:128, 0:256],
                   func=AF.Copy)
            elif cb < 9:
                g = cb - 5
                op("act", "activation", r=[bn], w=["ug%d" % g], out=ug[g][:, 16:272], in_=pz[:, 0:256],
                   func=AF.Copy)
            elif cb < 13:
                j = cb - 9
                op("act", "activation", r=[bn], w=["qiTz"], out=qiTz[0:64, j, :], in_=pz[0:64, 0:256], func=AF.Copy)
                op("act", "activation", r=[bn], w=["qiTz"], out=qiTz[64:128, 4 + j, :], in_=pz[64:128, 0:256],
                   func=AF.Copy)
            else:
                op("act", "activation", r=[bn], w=["kiT"], out=kiT[:, t0:t0 + 256], in_=pz[:, 0:256], func=AF.Copy)
            yield cb
        if tokmajor:
            for st in range(2):
                blk = pr * 2 + st
                bank = bi % 2
                bi += 1
                bn = "ps%d" % bank
                pz = ps[bank]
                for kc in range(8):
                    op("pe", "matmul", pz[:, 0:NC_TM], r=["Wtm", xn], w=[bn],
                       lhsT=xt[:, kc, st * 128:(st + 1) * 128], rhs=Wtm[:, kc, :], start=(kc == 0),
                       stop=(kc == 7))
                op("act", "activation", r=[bn], w=["Vx"], out=Vx[:, blk, :, 0:64],
                   in_=pz[:, 0:128].rearrange("p (g c) -> p g c", g=2), func=AF.Copy)
                op("act", "activation", r=[bn], w=["wI"], out=wI[:, blk, :], in_=pz[:, 128:136], func=AF.Copy)
                yield 100 + st

    def proj_blocks(pr, cbs, tokmajor):
        for _ in proj_gen(pr, cbs, tokmajor):
            pass

    def chain(*gens):
        for g_ in gens:
            if g_ is not None:
                for v_ in g_:
                    yield v_

    def pooling_gen(pr):
        for g, win in enumerate(POOL_WINDOWS):
            un = "ug%d" % g
            u = ug[g]
            cur, curn = u, un
            sh = 1
            for lvl in range(g + 1):
                dst, dstn = (pa, "pa") if lvl % 2 == 0 else (pb, "pb")
                op("pool", "tensor_tensor", r=[curn], w=[dstn], out=dst[:, sh:272], in0=cur[:, sh:272],
                   in1=cur[:, 0:272 - sh], op=ALU.add)
                cur, curn = dst, dstn
                sh *= 2
            if pr == 0:
                op("pool", "tensor_tensor", r=[curn, "mrc"], w=[curn], out=cur[:, 16:32], in0=cur[:, 16:32],
                   in1=mrc[:, g, :], op=ALU.mult)
            yield 0
            dn = "dTg%d" % (g % 2)
            dt_ = dTg[g % 2]
            op("dve", "scalar_tensor_tensor", r=[un, curn], w=[dn], out=dt_[:], in0=u[:, 16:272],
               scalar=-float(win), in1=cur[:, 16:272], op0=ALU.mult, op1=ALU.add)
            yield 1
            op("pool", "tensor_copy", r=[un], w=[un], out=u[:, 0:16], in_=u[:, 256:272])
            bank = 7
            bn = "ps%d" % bank
            op("pe", "matmul", ps[bank][:, 0:256], r=["wpool", dn], w=[bn], lhsT=wpool[:, g, :], rhs=dt_[:],
               start=True, stop=True)
            op("act", "activation", r=[bn, "psc"], w=["poolTt"], out=poolTt[:, g, :], in_=ps[bank][:, 0:256],
               func=AF.Identity, scale=psc[:, g:g + 1])
            yield 3

    def acc_gen(qb):
        qq = qb % 2
        nk = (qb + 1) * 128
        tq = slice(qq * 128, (qq + 1) * 128)
        scores = SC[qb % 2]
        scn = "sc%d" % (qb % 2)
        nch = (nk + 511) // 512
        tiles = [(c, h) for c in range(nch) for h in range(8)]

        def f_mm(i):
            c, h = tiles[i]
            k0 = c * 512
            wdt = min(512, nk - k0)
            bank = i % 2
            op("pe", "matmul", ps[bank][:, 0:wdt], r=["qiTz", "kiT"], w=["ps%d" % bank], lhsT=qiTz[:, h, tq],
               rhs=kiT[:, k0:k0 + wdt], start=True, stop=True)

        def f_relu(i):
            c, h = tiles[i]
            k0 = c * 512
            wdt = min(512, nk - k0)
            bank = i % 2
            op("act", "activation", r=["ps%d" % bank], w=["R%d" % (i % 3)], out=Rb[i % 3][:, 0:wdt],
               in_=ps[bank][:, 0:wdt], func=AF.Relu)

        def back(i):
            c, h = tiles[i]
            k0 = c * 512
            wdt = min(512, nk - k0)
            rb = Rb[i % 3]
            rn = "R%d" % (i % 3)
            if h == 0:
                op("dve", "tensor_scalar", r=[rn, "wI"], w=[scn], out=scores[:, k0:k0 + wdt],
                   in0=rb[:, 0:wdt], scalar1=wI[:, qb, h:h + 1], scalar2=None, op0=ALU.mult)
            else:
                op("dve", "scalar_tensor_tensor", r=[rn, "wI", scn], w=[scn],
                   out=scores[:, k0:k0 + wdt], in0=rb[:, 0:wdt], scalar=wI[:, qb, h:h + 1],
                   in1=scores[:, k0:k0 + wdt], op0=ALU.mult, op1=ALU.add)

        nt = len(tiles)
        for i in range(nt + 3):
            if i < nt:
                f_mm(i)
            if 0 <= i - 1 < nt:
                f_relu(i - 1)
            if 0 <= i - 3 < nt:
                back(i - 3)
            yield i

    def bis_gen(qb):
        nk = (qb + 1) * 128
        scores = SC[qb % 2]
        scn = "sc%d" % (qb % 2)
        MB = MBs[qb % 2]
        mbn, mbh = "MB%d" % (qb % 2), "MBh%d" % (qb % 2)
        sc = scores[:, 0:nk]
        SM = dict(r=["sm"], w=["sm"])
        op("dve", "tensor_reduce", r=[scn], w=["sm"], out=smc(MX), in_=sc, axis=AX.X, op=ALU.max)
        op("dve", "tensor_reduce", r=[scn], w=["sm"], out=smc(MN), in_=sc, axis=AX.X, op=ALU.min)
        dg = scores[:, qb * 128:(qb + 1) * 128]
        op("dve", "tensor_tensor", r=[scn, "cbias"], w=[scn], out=dg, in0=dg, in1=cbias[:], op=ALU.add)
        op("dve", "tensor_scalar", r=["iop1"], w=["sm"], out=smc(KT), in0=iop1[:], scalar1=float(qb * 128),
           scalar2=float(TOPK), op0=ALU.add, op1=ALU.min)
        op("dve", "tensor_tensor", out=smc(WD), in0=smc(MX), in1=smc(MN), op=ALU.subtract, **SM)
        op("dve", "tensor_scalar", out=smc(WD), in0=smc(WD), scalar1=1.0 + 1.0 / 1024, scalar2=1e-12,
           op0=ALU.mult, op1=ALU.add, **SM)
        op("dve", "tensor_scalar", r=["sm", "CV"], w=["WDS"], out=WDS[:, 0:n_bisect + 1], in0=CV[:, 0:n_bisect + 1],
           scalar1=smc(WD), scalar2=None, op0=ALU.mult)
        op("dve", "tensor_tensor", r=["sm", "WDS"], w=["sm"], out=smc(MID), in0=smc(MN), in1=WDS[:, 1:2], op=ALU.add)
        yield 0
        for itb in range(1, n_bisect + 1):
            op("dve", "tensor_scalar", r=[scn, "sm"], w=[mbn, "sm"], out=MB[:, 0:nk], in0=sc,
               scalar1=smc(MID), scalar2=0.0, op0=ALU.is_ge, op1=ALU.add, accum_out=smc(CNT))
            op("dve", "tensor_scalar", out=smc(GE), in0=smc(CNT), scalar1=smc(KT), scalar2=0.5, op0=ALU.is_ge,
               op1=ALU.subtract, **SM)
            if itb < n_bisect:
                op("dve", "scalar_tensor_tensor", r=["sm", "WDS"], w=["sm"], out=smc(MID), in0=smc(GE),
                   scalar=WDS[:, itb:itb + 1], in1=smc(MID), op0=ALU.mult, op1=ALU.add)
            yield itb
        op("dve", "tensor_scalar", r=["sm", "WDS"], w=["sm"], out=smc(TMP), in0=smc(GE), scalar1=-1.0,
           scalar2=WDS[:, n_bisect:n_bisect + 1], op0=ALU.add, op1=ALU.mult)
        op("dve", "tensor_tensor", out=smc(LO), in0=smc(TMP), in1=smc(MID), op=ALU.add, **SM)
        op("dve", "tensor_scalar", r=[scn, "sm"], w=[mbn, mbh], out=MB[:, 0:nk], in0=sc, scalar1=smc(LO),
           scalar2=NEG_MASK, op0=ALU.is_lt, op1=ALU.mult)
        yield 99

    def bis_gen_act(qb, nh=8):
        nk = (qb + 1) * 128
        scores = SC[qb % 2]
        scn = "sc%d" % (qb % 2)
        MB = MBs[qb % 2]
        mbn, mbh = "MB%d" % (qb % 2), "MBh%d" % (qb % 2)
        sc = scores[:, 0:nk]
        SM = dict(r=["sm"], w=["sm"])
        op("dve", "tensor_reduce", r=[scn], w=["sm"], out=smc(MX), in_=sc, axis=AX.X, op=ALU.max)
        op("dve", "tensor_reduce", r=[scn], w=["sm"], out=smc(MN), in_=sc, axis=AX.X, op=ALU.min)
        dg = scores[:, qb * 128:(qb + 1) * 128]
        op("dve", "tensor_tensor", r=[scn, "cbias"], w=[scn], out=dg, in0=dg, in1=cbias[:], op=ALU.add)
        op("dve", "tensor_scalar", r=["iop1"], w=["sm"], out=smc(KT), in0=iop1[:], scalar1=float(qb * 128),
           scalar2=float(TOPK), op0=ALU.add, op1=ALU.min)
        op("dve", "tensor_scalar", r=["sm"], w=["smb"], out=smc(BB), in0=smc(KT), scalar1=-2.0,
           scalar2=float(nk) + 0.5, op0=ALU.mult, op1=ALU.add)
        op("dve", "tensor_tensor", out=smc(WD), in0=smc(MX), in1=smc(MN), op=ALU.subtract, **SM)
        op("dve", "tensor_scalar", out=smc(WD), in0=smc(WD), scalar1=1.0 + 1.0 / 1024, scalar2=1e-12,
           op0=ALU.mult, op1=ALU.add, **SM)
        op("dve", "tensor_scalar", r=["sm", "CV"], w=["WDS"], out=WDS[:, 0:n_bisect + 1], in0=CV[:, 0:n_bisect + 1],
           scalar1=smc(WD), scalar2=None, op0=ALU.mult)
        op("dve", "tensor_scalar", r=["sm", "CV"], w=["NWDS"], out=NWDS[:, 0:n_bisect + 1],
           in0=CV[:, 0:n_bisect + 1], scalar1=smc(WD), scalar2=-1.0, op0=ALU.mult, op1=ALU.mult)
        op("dve", "tensor_tensor", r=["sm", "WDS"], w=["sm"], out=smc(MID), in0=smc(MN), in1=WDS[:, 1:2], op=ALU.add)
        nm = [smc(NMID), smc(NMID2)]
        op("dve", "tensor_scalar", r=["sm"], w=["nmid1"], out=nm[1], in0=smc(MID), scalar1=-1.0, scalar2=None,
           op0=ALU.mult)
        yield 0
        for itb in range(1, n_bisect + 1):
            cur, nxt = itb % 2, (itb + 1) % 2
            if itb % 2 == 1:
                op("dve", "tensor_scalar", r=[scn, "smmid"] if itb > 1 else [scn, "sm"], w=[mbn, "sm"],
                   out=MB[:, 0:nk], in0=sc, scalar1=smc(MID), scalar2=0.0, op0=ALU.is_ge, op1=ALU.add,
                   accum_out=smc(CNT))
                op("dve", "tensor_scalar", out=smc(GE), in0=smc(CNT), scalar1=smc(KT), scalar2=0.5, op0=ALU.is_ge,
                   op1=ALU.subtract, **SM)
                op("dve", "scalar_tensor_tensor", r=["sm", "NWDS", "nmid%d" % cur], w=["nmid%d" % nxt],
                   out=nm[nxt], in0=smc(GE), scalar=NWDS[:, itb:itb + 1], in1=nm[cur], op0=ALU.mult, op1=ALU.add)
            else:
                op("act", "activation", r=[scn, "nmid%d" % cur], w=[mbh, "smc2"], out=MB[:, 0:nk], in_=sc,
                   func=AF.Sign, bias=nm[cur], scale=1.0, accum_out=smc(CNT2))
                op("act", "activation", r=["smc2", "smb"], w=["smg"], out=smc(GE2), in_=smc(CNT2), func=AF.Sign,
                   bias=smc(BB), scale=1.0)
                if itb < n_bisect:
                    op("act", "activation", r=["smg", "NWDS", "nmid%d" % cur], w=["nmid%d" % nxt], out=nm[nxt],
                       in_=smc(GE2), func=AF.Identity, bias=nm[cur], scale=NWDS[:, itb + 1:itb + 2])
                    op("act", "activation", r=["nmid%d" % nxt], w=["smmid"], out=smc(MID), in_=nm[nxt],
                       func=AF.Identity, scale=-1.0)
            yield itb
        assert n_bisect % 2 == 0
        cur = n_bisect % 2
        op("dve", "tensor_scalar", r=["smg"], w=["sm"], out=smc(TMP), in0=smc(GE2), scalar1=0.5, scalar2=-1.0,
           op0=ALU.mult, op1=ALU.add)
        op("dve", "scalar_tensor_tensor", r=["sm", "WDS", "nmid%d" % cur], w=["sm"], out=smc(LO), in0=smc(TMP),
           scalar=WDS[:, n_bisect:n_bisect + 1], in1=nm[cur], op0=ALU.mult, op1=ALU.subtract)
        op("dve", "tensor_scalar", r=[scn, "sm", mbh], w=[mbn, mbh], out=MB[:, 0:nk], in0=sc, scalar1=smc(LO),
           scalar2=NEG_MASK, op0=ALU.is_lt, op1=ALU.mult)
        yield 99

    def attn_units(qb):
        qq = qb % 2
        tq = slice(qq * 128, (qq + 1) * 128)
        MB = MBs[qb % 2]
        mbn, mbh = "MB%d" % (qb % 2), "MBh%d" % (qb % 2)
        units = [(sb_, g) for sb_ in range(qb + 1) for g in range(2)]

        def qkmm(i):
            sb_, g = units[i]
            bank = 2 + (i % 2)
            bn = "ps%d" % bank
            op("pe", "matmul", ps[bank][:, :], r=["kTz", "qTt"], w=[bn], lhsT=kTz[:, g, sb_ * 128:(sb_ + 1) * 128],
               rhs=qTt[:, :, tq], start=True, stop=False)
            op("pe", "matmul", ps[bank][:, :], r=[mbn, mbh, "I4"], w=[bn], lhsT=MB[:, sb_ * 128:(sb_ + 1) * 128],
               rhs=I4[:, :, :], start=False, stop=True)

        def ex(i):
            bank = 2 + (i % 2)
            op("act", "activation", r=["ps%d" % bank], w=["PT%d" % (i % 3)], out=PT[i % 3][:], in_=ps[bank][:, :],
               func=AF.Exp, scale=0.125)

        def pv(i):
            sb_, g = units[i]
            bank = 4 + g
            op("pe", "matmul", ps[bank][0:65, :], r=["Vx", "PT%d" % (i % 3)], w=["ps%d" % bank],
               lhsT=Vx[:, sb_, g, :], rhs=PT[i % 3][:], start=(sb_ == 0), stop=(sb_ == qb))

        nu = len(units)
        qkmm(0)
        if nu > 1:
            qkmm(1)
        ex(0)
        for i in range(nu):
            if i + 2 < nu:
                qkmm(i + 2)
            if i + 1 < nu:
                ex(i + 1)
            pv(i)
            yield i
        for g in range(2):
            op("act", "activation", r=["ps%d" % (4 + g)], w=["OTs"], out=OTs[0:65, g, :],
               in_=ps[4 + g][0:65, :], func=AF.Copy)
        yield nu

    def tail_gen(qb):
        qq = qb % 2
        tq = slice(qq * 128, (qq + 1) * 128)
        zn = "z%d" % qq
        zt = z[:, qq, :]
        for g in range(2):
            bank = 6 + g
            for j in range(4):
                op("pe", "transpose", ps[bank][:, j * 66:j * 66 + 65], OTs[0:65, g, j * 128:(j + 1) * 128],
                   ident32[0:65, 0:65], r=["OTs", "ident32"], w=["ps%d" % bank])
        yield 1
        for g in range(2):
            bank = 6 + g
            pv_ = ps[bank][:, 0:264].rearrange("p (j c) -> p j c", j=4)
            op("dve", "reciprocal", r=["ps%d" % bank], w=["rt"], out=rt[:, g * 4:(g + 1) * 4], in_=pv_[:, :, 64])
            op("dve", "tensor_tensor", r=["ps%d" % bank, "rt"], w=["Otok"], out=Otok[:, g * 4:(g + 1) * 4, :],
               in0=pv_[:, :, 0:64], in1=rt[:, g * 4:(g + 1) * 4].unsqueeze(2).to_broadcast([128, 4, 64]),
               op=ALU.mult)
        yield 2
        p6 = ps[6].bitcast(BF16)
        for c in range(4):
            op("pe", "transpose", p6[:, c * 128:(c + 1) * 128],
               Otok[:, 2 * c:2 * c + 2, :].rearrange("p a b -> p (a b)"), I4[:, 0, :],
               r=["Otok", "I4"], w=["ps6"])
        yield 3
        op("act", "activation", r=["ps6"], w=["catT"], out=catT[:].rearrange("p a b -> p (a b)"),
           in_=p6[:, 0:512], func=AF.Copy)
        yield 4
        for dh in range(2):
            bank = 6 + dh
            for c in range(8):
                lt = catT[:, c, :] if c < 4 else poolTt[:, c - 4, tq]
                op("pe", "matmul", ps[bank][:, :], r=["catT", "poolTt", "Wout"], w=["ps%d" % bank], lhsT=lt,
                   rhs=Wout[:, c, dh * 512:(dh + 1) * 512], start=(c == 0), stop=(c == 7))
        yield 5
        for dh in range(2):
            zs = z[:, qq, dh * 512:(dh + 1) * 512]
            op("dve", "scalar_tensor_tensor", r=[zn, "ps%d" % (6 + dh)], w=[zn], out=zs, in0=zs, scalar=ALPHA,
               in1=ps[6 + dh][:, :], op0=ALU.mult, op1=ALU.add)
        for dh in range(2):
            op("dve", "bn_stats", r=[zn], w=["st6"], out=st6[:, dh, :], in_=zt[:, dh * 512:(dh + 1) * 512])
        op("dve", "bn_aggr", r=["st6"], w=["sm"], out=sm[:, 24:26], in_=st6[:].rearrange("p a b -> p (a b)"))
        op("dve", "tensor_scalar", r=["sm"], w=["sm"], out=smc(TMP), in0=sm[:, 25:26], scalar1=LN_EPS,
           scalar2=None, op0=ALU.add)
        yield 6
        op("pool", "tensor_tensor", r=["sm", "negh"], w=["sm2"], out=smc(RSTD), in0=smc(TMP), in1=negh[:],
           op=ALU.pow)
        yield 7
        op("dve", "scalar_tensor_tensor", r=["sm", "sm2"], w=["sm3"], out=smc(NMR), in0=sm[:, 24:25], scalar=-1.0,
           in1=smc(RSTD), op0=ALU.mult, op1=ALU.mult)
        yield 8
        op("act", "activation", r=[zn, "sm2", "sm3"], w=[zn], out=zt, in_=zt, func=AF.Identity, bias=smc(NMR),
           scale=smc(RSTD))
        yield 9
        op("dve", "tensor_tensor", r=[zn, "lnp"], w=[zn], out=zt, in0=zt, in1=lnp[:, 0, :], op=ALU.mult)
        op("dve", "tensor_tensor", r=[zn, "lnp"], w=[zn], out=zt, in0=zt, in1=lnp[:, 1, :], op=ALU.add)
        yield 10
        for half in range(2):
            bank = 6 + half
            for k4 in range(4):
                dc = half * 4 + k4
                op("pe", "transpose", ps[bank][:, k4 * 128:(k4 + 1) * 128], z[:, qq, dc * 128:(dc + 1) * 128],
                   ident32[:], r=[zn, "ident32"], w=["ps%d" % bank])
        yield 11
        for half in range(2):
            pview = ps[6 + half][:, :].rearrange("p (k t) -> p k t", k=4)
            op("act", "activation", r=["ps%d" % (6 + half)], w=["gu1"], out=hT32[:, half * 4:(half + 1) * 4, :],
               in_=pview, func=AF.Copy)
        yield 12
        for half in range(2):
            op("dve", "tensor_copy", r=["gu1"], w=["hTb"], out=hTb[:, half * 4:(half + 1) * 4, tq],
               in_=hT32[:, half * 4:(half + 1) * 4, :])
        for dc in range(8):
            op("pe", "matmul", ps[6][:, 0:20], r=["gu1", "wr"], w=["ps6"], lhsT=hT32[:, dc, :],
               rhs=wr[:, dc, :], start=(dc == 0), stop=(dc == 7))
        yield 13
        LG = rt[:, 16:36]
        GL = rt[:, 16:20]
        EL = rt[:, 20:36].rearrange("p (g e) -> p g e", g=4)
        GMAX, GNEG, GSUM, GW = rt[:, 36:37], rt[:, 37:38], rt[:, 38:39], rt[:, 39:40]
        GOH = rt[:, 40:44]
        TM16 = rt[:, 44:60]
        ES = rt[:, 60:64]
        M1, M2, DDIF, ED, W1, W2 = (rt[:, 64 + i:65 + i] for i in range(6))
        OH1, ES2, OH2, IG = rt[:, 72:76], rt[:, 76:80], rt[:, 80:84], rt[:, 84:88]
        GEX = rt[:, 88:92]
        RT = dict(r=["rt"], w=["rt"])
        op("dve", "tensor_tensor", r=["ps6", "br"], w=["rt"], out=LG, in0=ps[6][:, 0:20], in1=br[:], op=ALU.add)
        op("dve", "tensor_reduce", out=GMAX, in_=GL, axis=AX.X, op=ALU.max, **RT)
        op("dve", "tensor_scalar", out=GNEG, in0=GMAX, scalar1=-1.0, scalar2=None, op0=ALU.mult, **RT)
        op("dve", "tensor_scalar", out=GOH, in0=GL, scalar1=GMAX, scalar2=None, op0=ALU.is_equal, **RT)
        op("dve", "tensor_tensor", out=TM16.rearrange("p (g e) -> p g e", g=4), in0=EL,
           in1=GOH.unsqueeze(2).to_broadcast([128, 4, 4]), op=ALU.mult, **RT)
        op("dve", "tensor_reduce", out=ES, in_=TM16.rearrange("p (g e) -> p e g", g=4), axis=AX.X, op=ALU.add,
           **RT)
        op("dve", "tensor_reduce", out=M1, in_=ES, axis=AX.X, op=ALU.max, **RT)
        op("dve", "tensor_scalar", out=OH1, in0=ES, scalar1=M1, scalar2=None, op0=ALU.is_equal, **RT)
        op("dve", "scalar_tensor_tensor", out=ES2, in0=OH1, scalar=-1e30, in1=ES, op0=ALU.mult, op1=ALU.add,
           **RT)
        op("dve", "tensor_reduce", out=M2, in_=ES2, axis=AX.X, op=ALU.max, **RT)
        op("dve", "tensor_scalar", out=OH2, in0=ES2, scalar1=M2, scalar2=None, op0=ALU.is_equal, **RT)
        op("dve", "tensor_tensor", out=DDIF, in0=M2, in1=M1, op=ALU.subtract, **RT)
        yield 14
        op("act", "activation", r=["rt"], w=["rt2"], out=GEX, in_=GL, func=AF.Exp, bias=GNEG, scale=1.0,
           accum_out=GSUM)
        op("act", "activation", r=["rt"], w=["rt3"], out=ED, in_=DDIF, func=AF.Exp)
        yield 15
        op("dve", "reciprocal", r=["rt2"], w=["rt"], out=GW, in_=GSUM)
        op("dve", "tensor_scalar", r=["rt3"], w=["rt"], out=W1, in0=ED, scalar1=1.0, scalar2=None, op0=ALU.add)
        op("dve", "reciprocal", out=W1, in_=W1, **RT)
        op("dve", "tensor_tensor", out=W1, in0=W1, in1=GW, op=ALU.mult, **RT)
        op("dve", "tensor_tensor", r=["rt", "rt3"], w=["rt"], out=W2, in0=W1, in1=ED, op=ALU.mult)
        op("dve", "tensor_scalar", out=IG, in0=OH1, scalar1=W1, scalar2=None, op0=ALU.mult, **RT)
        op("dve", "scalar_tensor_tensor", out=IG, in0=OH2, scalar=W2, in1=IG, op0=ALU.mult, op1=ALU.add, **RT)
        op("dve", "tensor_tensor", r=["rt"], w=["gate"], out=gate[:, qq, :].rearrange("p (g e) -> p g e", g=4),
           in0=GOH.unsqueeze(2).to_broadcast([128, 4, 4]), in1=IG.unsqueeze(1).to_broadcast([128, 4, 4]),
           op=ALU.mult)
        yield 16

    def moe_gen(pr):
        t0 = pr * 256
        unitsm = [(ex, st) for ex in range(N_EXP) for st in range(2)]
        nm_ = len(unitsm)
        load_gu(1)

        def m_ab_mm(i):
            ex, st = unitsm[i]
            s_ = ex % 2
            bank = 4 + (i % 2)
            bn = "ps%d" % bank
            for kc in range(8):
                op("pe", "matmul", ps[bank][:, :], r=["hTb", "gu%d" % s_], w=[bn],
                   lhsT=hTb[:, kc, st * 128:(st + 1) * 128], rhs=gu[s_][:, kc, :], start=(kc == 0), stop=(kc == 7))

        def m_ab_ew(i):
            ex, st = unitsm[i]
            bank = 4 + (i % 2)
            bn = "ps%d" % bank
            op("act", "activation", r=[bn], w=["sa%d" % (i % 2)], out=sa[i % 2][:], in_=ps[bank][:, 0:256],
               func=AF.Silu)
            op("dve", "scalar_tensor_tensor", r=[bn, "gate", "sa%d" % (i % 2)], w=["hid%d" % (i % 2)],
               out=hid[i % 2][:], in0=ps[bank][:, 256:512], scalar=gate[:, st, ex:ex + 1], in1=sa[i % 2][:],
               op0=ALU.mult, op1=ALU.mult)

        def m_t(i):
            bank = 6 + (i % 2)
            pb_ = ps[bank].bitcast(BF16)
            for fc in range(2):
                op("pe", "transpose", pb_[:, fc * 128:(fc + 1) * 128], hid[i % 2][:, fc * 128:(fc + 1) * 128],
                   I4[:, 0, :], r=["hid%d" % (i % 2), "I4"], w=["ps%d" % bank])
            op("act", "activation", r=["ps%d" % bank], w=["hidT%d" % (i % 2)],
               out=hidT[i % 2][:].rearrange("p a b -> p (a b)"), in_=pb_[:, 0:256], func=AF.Copy)

        def m_d(i):
            ex, st = unitsm[i]
            s_ = ex % 2
            for dh in range(2):
                bank = st * 2 + dh
                for fc in range(2):
                    op("pe", "matmul", ps[bank][:, :], r=["hidT%d" % (i % 2), "dd%d" % s_], w=["ps%d" % bank],
                       lhsT=hidT[i % 2][:, fc, :], rhs=dd[s_][:, fc, dh * 512:(dh + 1) * 512],
                       start=(ex == 0 and fc == 0), stop=(ex == N_EXP - 1 and fc == 1))

        for rnd in range(nm_ + 4):
            if rnd % 2 == 0 and 2 <= rnd < nm_:
                ex = rnd // 2
                if ex + 1 < N_EXP:
                    load_gu(ex + 1)
            if rnd < nm_:
                m_ab_mm(rnd)
            if 0 <= rnd - 1 < nm_:
                m_ab_ew(rnd - 1)
            if 0 <= rnd - 2 < nm_:
                m_t(rnd - 2)
            if 0 <= rnd - 3 < nm_:
                m_d(rnd - 3)
            if rnd % 2 == 0 and 2 <= rnd - 2 < nm_:
                ex = (rnd - 2) // 2
                if ex + 1 < N_EXP:
                    load_dd(ex + 1)
            yield rnd

    def moe_fin_gen(pr, zp):
        t0 = pr * 256
        MV, TMP2, RSTD2, NMR2 = sm[:, 28:30], sm[:, 27:28], sm[:, 30:31], sm[:, 31:32]
        for st in range(2):
            zn = "z%d" % st
            zt = z[:, st, :]
            if st == 0:
                for dh in range(2):
                    bank = dh
                    zs = z[:, 0, dh * 512:(dh + 1) * 512]
                    op("dve", "scalar_tensor_tensor", r=["z0", "ps%d" % bank], w=["z0"], out=zs, in0=zs,
                       scalar=ALPHA, in1=ps[bank][:, :], op0=ALU.mult, op1=ALU.add)
                for dh in range(2):
                    bank = 2 + dh
                    zs = z[:, 1, dh * 512:(dh + 1) * 512]
                    op("dve", "scalar_tensor_tensor", r=["z1", "ps%d" % bank], w=["z1"], out=zs, in0=zs,
                       scalar=ALPHA, in1=ps[bank][:, :], op0=ALU.mult, op1=ALU.add)
            for dh in range(2):
                op("dve", "bn_stats", r=[zn], w=["st6b"], out=st6b[:, 0, dh * 6:(dh + 1) * 6],
                   in_=zt[:, dh * 512:(dh + 1) * 512])
            op("dve", "bn_aggr", r=["st6b"], w=["smf"], out=MV, in_=st6b[:, 0, :])
            op("dve", "tensor_scalar", r=["smf"], w=["smf"], out=TMP2, in0=sm[:, 29:30], scalar1=LN_EPS,
               scalar2=None, op0=ALU.add)
            yield 0
            op("pool", "tensor_tensor", r=["smf", "negh"], w=["smf2"], out=RSTD2, in0=TMP2, in1=negh[:], op=ALU.pow)
            yield 1
            op("dve", "scalar_tensor_tensor", r=["smf", "smf2"], w=["smf3"], out=NMR2, in0=sm[:, 28:29], scalar=-1.0,
               in1=RSTD2, op0=ALU.mult, op1=ALU.mult)
            yield 2
            op("act", "activation", r=[zn, "smf2", "smf3"], w=[zn], out=zt, in_=zt, func=AF.Identity, bias=NMR2,
               scale=RSTD2)
            yield 3
            op("dve", "tensor_tensor", r=[zn, "lnp"], w=[zn], out=zt, in0=zt, in1=lnp[:, 2, :], op=ALU.mult)
            op("dve", "tensor_tensor", r=[zn, "lnp"], w=[zn], out=zt, in0=zt, in1=lnp[:, 3, :], op=ALU.add)
            yield 4
            P.dma("sync", "out%d" % st, out_d[t0 + st * 128:t0 + (st + 1) * 128, :], zt, r=[zn], w=["o%d" % st])
            if 0 <= zp < npairs:
                P.dma("sync", "z%d" % st, zt, x_d[zp * 256 + st * 128:zp * 256 + (st + 1) * 128, :], w=[zn])
            yield 5
        if 0 <= zp < npairs:
            load_gu(0); load_dd(0); load_dd(1)
        yield 6

    def idle(n):
        for _ in range(n):
            yield 0

    IDX_BLOCKS = [9, 10, 11, 12, 13]
    MIX_BLOCKS = [0, 1, 2, 3, 4, 5, 6, 7, 8]
    nblk = 2 * npairs

    def run_threads(threads):
        active = [t for t in threads if t[0] is not None]
        while active:
            maxc = max(t[1] for t in active)
            for jj in range(maxc):
                for t in list(active):
                    if t[1] > jj and t in active:
                        try:
                            next(t[0])
                        except StopIteration:
                            active.remove(t)

    def per(n, ticks=16):
        return max(1, -(-n // ticks))

    proj_blocks(0, IDX_BLOCKS, True)
    run_threads([[acc_gen(0), 1]])
    for k in range(nblk + 3):
        bg = None
        if k < nblk:
            bg = bis_gen(k) if (k % 2 == 0 and 0 <= k // 2 - 2 < npairs) else bis_gen_act(k)
        lg = tail_gen(k - 2) if 0 <= k - 2 < nblk else None
        if k % 2 == 1:
            p1, p0 = (k + 1) // 2, (k - 1) // 2
            pj = chain(proj_gen(p1, IDX_BLOCKS, True) if p1 < npairs else None,
                       proj_gen(p0, MIX_BLOCKS, False) if p0 < npairs else None)
            act_ = [[lg, 1], [bg, 1], [lg, 1], [pj, 4]]
            while True:
                live = False
                for t_ in act_:
                    if t_[0] is None:
                        continue
                    for _ in range(t_[1]):
                        try:
                            next(t_[0])
                            if t_[0] is pj:
                                live = True
                        except StopIteration:
                            if t_[0] is pj:
                                live = False
                            t_[0] = None if t_[0] is pj else t_[0]
                            break
                if not live:
                    break
            if p0 < npairs and p0 + 2 < npairs:
                load_xT(p0 + 2)
        else:
            j = k // 2 - 2
            zp = k // 2 - 1
            fg = None
            if 0 <= j < npairs:
                run_threads([[moe_gen(j), 2], [bg, 1]])
                bg = None
                fg = moe_fin_gen(j, zp)
                if lg is not None:
                    lg = chain(idle(6), lg)
            elif 0 <= zp < npairs:
                for qq in range(2):
                    P.dma("sync", "z%d" % qq, z[:, qq, :], x_d[zp * 256 + qq * 128:zp * 256 + (qq + 1) * 128, :],
                          w=["z%d" % qq])
                load_gu(0); load_dd(0); load_dd(1)
        ag = acc_gen(k + 1) if k + 1 < nblk else None
        tg = attn_units(k - 1) if 0 <= k - 1 < nblk else None
        nt_acc = 8 * (((k + 2) * 128 + 511) // 512) + 2 if ag is not None else 0
        pg = pooling_gen((k - 1) // 2) if (k % 2 == 1 and (k - 1) // 2 < npairs) else None
        fg_ = fg if k % 2 == 0 else None
        run_threads([[fg_, 1], [lg, 1], [ag, per(nt_acc, 18)], [bg, 1], [fg_, 1], [lg, 1], [tg, per(2 * k, 18)], [pg, 1]])
        if k == 1:
            for ex in range(4, 8):
                wcast(ex)
        if k == 2:
            for ex in range(8, N_EXP):
                wcast(ex)

    P.wait_all("sync", [P.lastw[k] for k in ("o0", "o1") if k in P.lastw])

    with nc.Block() as block:
        @block.sync
        def _(e):
            P.replay("sync", e)

        @block.scalar
        def _(e):
            P.replay("act", e)

        @block.vector
        def _(e):
            P.replay("dve", e)

        @block.gpsimd
        def _(e):
            P.replay("pool", e)

        @block.tensor
        def _(e):
            P.replay("pe", e)
    stack.close()
    return nc


def _prep_inputs(inp):
    f = np.float32
    w_in = np.asarray(inp["w_in"], f)[0]
    q = w_in[:, 0:512]; k = w_in[:, 512:640]; v = w_in[:, 640:768]; pool = w_in[:, 768:1280]
    qi = w_in[:, 1280:1792]; ki = w_in[:, 1792:1856]; wi = w_in[:, 1856:1864]
    cols = []
    for j in range(4):
        cols += [q[:, j * 64:(j + 1) * 64], q[:, (4 + j) * 64:(5 + j) * 64]]
    cols.append(k)
    cols.append(pool)
    for j in range(4):
        cols += [qi[:, j * 64:(j + 1) * 64], qi[:, (4 + j) * 64:(5 + j) * 64]]
    cols += [ki, ki, v, wi]
    w_in_p = np.ascontiguousarray(np.concatenate(cols, axis=1))
    assert w_in_p.shape == (D, NCOLS)
    lnp = np.ascontiguousarray(np.stack([np.asarray(inp[n], f)[0] for n in ("ln1_g", "ln1_b", "ln2_g", "ln2_b")]))
    w_router = np.ascontiguousarray(np.concatenate([np.asarray(inp["w_group_router"], f)[0],
                                                    np.asarray(inp["w_expert_router"], f)[0]], axis=1))
    b_router = np.ascontiguousarray(np.concatenate([np.asarray(inp["b_group_router"], f)[0],
                                                    np.asarray(inp["b_expert_router"], f)[0]]))
    wg = np.asarray(inp["w_gate"], f)[0].reshape(N_EXP, D, 256)
    wu = np.asarray(inp["w_up"], f)[0].reshape(N_EXP, D, 256)
    w_gu = np.ascontiguousarray(np.concatenate([wg, wu], axis=2))
    w_d = np.ascontiguousarray(np.asarray(inp["w_down"], f)[0].reshape(N_EXP, 256, D))
    shared = {
        "w_in_p": w_in_p,
        "w_pool": np.ascontiguousarray(np.asarray(inp["w_pool"], f)[0]),
        "pool_scale": np.ascontiguousarray(np.asarray(inp["pool_scale"], f)[0]),
        "w_out": np.ascontiguousarray(np.asarray(inp["w_out"], f)[0]),
        "ln_params": lnp, "w_router": w_router, "b_router": b_router, "w_gu": w_gu, "w_d": w_d,
    }
    x = np.asarray(inp["x"], f)
    maps = []
    for b in range(x.shape[0]):
        m = dict(shared)
        m["x"] = np.ascontiguousarray(x[b])
        m["xT"] = np.ascontiguousarray(x[b].T)
        maps.append(m)
    return maps


def kernel(**inputs):
    maps = _prep_inputs(inputs)
    nc = build_nc()
    res = run_bass_kernel_spmd(nc, maps, core_ids=list(range(8)))
    return np.stack([np.asarray(r["out"], np.float32) for r in res.results], axis=0)
```

```python
import numpy as np
from contextlib import ExitStack
import concourse.bass as bass
import concourse.mybir as mybir
from concourse.bass_utils import run_bass_kernel_spmd

F32 = mybir.dt.float32
BF16 = mybir.dt.bfloat16
I32 = mybir.dt.int32
MBDT = mybir.dt.bfloat16
AF = mybir.ActivationFunctionType
ALU = mybir.AluOpType
AX = mybir.AxisListType

S = 4096
D = 1024
NFM = 14
NC_FM = NFM * 128
NC_TM = 136
NCOLS = NC_FM + NC_TM
N_BISECT = 16
ALPHA = float(2.0 ** 0.25)
LN_EPS = 1e-5
TOPK = 256
NEG_MASK = -30000.0
N_EXP = 16
POOL_WINDOWS = (2, 4, 8, 16)


class Prog:
    ENGS = ("sync", "act", "dve", "pool", "pe")

    def __init__(self, nc, stack):
        self.nc = nc
        self.stack = stack
        self.q = {e: [] for e in self.ENGS}
        self.cnt = {e: 0 for e in self.ENGS}
        self.seen = {e: {} for e in self.ENGS}
        self.esem = {e: stack.enter_context(nc.semaphore("sem_" + e)) for e in self.ENGS}
        self.dsem = {}
        self.dcnt = {}
        self.lastw = {}
        self.readers = {}

    def _deps(self, r, w):
        deps = []
        for b in r:
            t = self.lastw.get(b)
            if t is not None:
                deps.append(t)
        for b in w:
            t = self.lastw.get(b)
            if t is not None:
                deps.append(t)
            deps.extend(self.readers.get(b, ()))
        return deps

    def _commit(self, tok, r, w):
        for b in r:
            self.readers.setdefault(b, []).append(tok)
        for b in w:
            self.lastw[b] = tok
            self.readers[b] = []

    def _waits(self, eng, deps):
        need = {}
        for t in deps:
            if t[0] == "e":
                if t[1] == eng and eng == "pe":
                    continue
                key = ("e", t[1]); sem = self.esem[t[1]]; val = t[2]
            else:
                key = ("d", t[1]); sem = self.dsem[t[1]]; val = t[2]
            if key not in need or need[key][1] < val:
                need[key] = (sem, val)
        out = []
        for key, (sem, val) in need.items():
            if self.seen[eng].get(key, 0) >= val:
                continue
            self.seen[eng][key] = val
            out.append((sem, val))
        return out

    def op(self, eng, name, *args, r=(), w=(), **kw):
        waits = self._waits(eng, self._deps(r, w))
        self.cnt[eng] += 1
        tok = ("e", eng, self.cnt[eng])
        self.q[eng].append((waits, (name, args, kw), tok))
        self._commit(tok, r, w)
        return tok

    def dma(self, eng, key, out, in_, r=(), w=(), **kw):
        if key not in self.dsem:
            self.dsem[key] = self.stack.enter_context(self.nc.semaphore("dsem_" + key))
            self.dcnt[key] = 0
        waits = self._waits(eng, self._deps(r, w))
        self.dcnt[key] += 16
        tok = ("d", key, self.dcnt[key])
        kw = dict(kw); kw["out"] = out; kw["in_"] = in_
        self.q[eng].append((waits, ("dma_start", (), kw), tok))
        self._commit(tok, r, w)
        return tok

    def wait_all(self, eng, toks):
        waits = self._waits(eng, toks)
        self.q[eng].append((waits, None, None))

    def replay(self, eng, e):
        for waits, fn, tok in self.q[eng]:
            for sem, val in waits:
                e.wait_ge(sem, val)
            if fn is None:
                continue
            name, args, kw = fn
            ins = getattr(e, name)(*args, **kw)
            if tok[0] == "e":
                ins.then_inc(self.esem[eng], 1)
            else:
                ins.then_inc(self.dsem[tok[1]], 16)


def build_nc(npairs=16, n_bisect=N_BISECT):
    nc = bass.Bass("TRN2", target_bir_lowering=False)
    stack = ExitStack()
    P = Prog(nc, stack)

    def dram(name, shape, dt, kind):
        return nc.dram_tensor(name, list(shape), dt, kind=kind).ap()

    def sb(name, shape, dt):
        return stack.enter_context(nc.sbuf_tensor(name, list(shape), dt))

    x_d = dram("x", [S, D], F32, "ExternalInput")
    xT_d = dram("xT", [D, S], F32, "ExternalInput")
    win_d = dram("w_in_p", [D, NCOLS], F32, "ExternalInput")
    wpool_d = dram("w_pool", [4, 128, 128], F32, "ExternalInput")
    psc_d = dram("pool_scale", [512], F32, "ExternalInput")
    wout_d = dram("w_out", [D, D], F32, "ExternalInput")
    lnp_d = dram("ln_params", [4, D], F32, "ExternalInput")
    wr_d = dram("w_router", [D, 20], F32, "ExternalInput")
    br_d = dram("b_router", [20], F32, "ExternalInput")
    wgu_d = dram("w_gu", [N_EXP, D, 512], F32, "ExternalInput")
    wd_d = dram("w_d", [N_EXP, 256, D], F32, "ExternalInput")
    out_d = dram("out", [S, D], F32, "ExternalOutput")
    wgu_bf = dram("w_gu_bf", [N_EXP, D, 512], BF16, "Internal")
    wd_bf = dram("w_d_bf", [N_EXP, 256, D], BF16, "Internal")
    wfm_bf = dram("w_fm_bf", [NFM, 128, 8, 128], BF16, "Internal")
    wtm_bf = dram("w_tm_bf", [128, 8, NC_TM], BF16, "Internal")

    Wb = [sb("Wb%d" % i, [128, 8, 128], BF16) for i in range(4)]
    Wtm = sb("Wtm", [128, 8, NC_TM], BF16)
    Wout = sb("Wout", [128, 8, D], BF16)
    wpool = sb("wpool", [128, 4, 128], BF16)
    psc = sb("psc", [128, 4], F32)
    lnp = sb("lnp", [128, 4, D], F32)
    wr = sb("wr", [128, 8, 20], F32)
    br = sb("br", [128, 20], F32)
    iot_i = sb("iot_i", [128, 128], I32)
    iot_f = sb("iot_f", [128, 128], F32)
    ident32 = sb("ident32", [128, 128], F32)
    I4 = sb("I4", [128, 4, 128], BF16)
    cbias = sb("cbias", [128, 128], F32)
    iop1 = sb("iop1", [128, 1], F32)
    mrc = sb("mrc", [128, 4, 16], F32)
    negh = sb("negh", [128, 1], F32)
    CV = sb("CV", [128, 32], F32)
    WDS = sb("WDS", [128, 32], F32)
    NWDS = sb("NWDS", [128, 32], F32)
    kTz = sb("kTz", [128, 2, S], BF16)
    kiT = sb("kiT", [128, S], BF16)
    Vx = sb("Vx", [128, 32, 2, 65], BF16)
    wI = sb("wI", [128, 32, 8], F32)
    xTt = [sb("xTt%d" % i, [128, 8, 256], BF16) for i in range(2)]
    qTt = sb("qTt", [128, 4, 256], BF16)
    qiTz = sb("qiTz", [128, 8, 256], BF16)
    ug = [sb("ug%d" % g, [128, 272], F32) for g in range(4)]
    pa = sb("pa", [128, 272], F32)
    pb = sb("pb", [128, 272], F32)
    dTg = [sb("dTg%d" % i, [128, 256], BF16) for i in range(2)]
    poolTt = sb("poolTt", [128, 4, 256], BF16)
    SC = [sb("scores%d" % i, [128, S], F32) for i in range(2)]
    MBs = [sb("MB%d" % i, [128, S], MBDT) for i in range(2)]
    Rb = [sb("R%d" % i, [128, 512], F32) for i in range(3)]
    PT = [sb("PT%d" % i, [128, 512], BF16) for i in range(3)]
    Otok = sb("Otok", [128, 8, 64], BF16)
    OTs = sb("OTs", [128, 2, 512], F32)
    catT = sb("catT", [128, 4, 128], BF16)
    z = sb("z", [128, 2, D], F32)
    hTb = sb("hTb", [128, 8, 256], BF16)
    gate = sb("gate", [128, 2, 16], F32)
    gu = [sb("gu%d" % i, [128, 8, 512], BF16) for i in range(2)]
    dd = [sb("dd%d" % i, [128, 2, D], BF16) for i in range(2)]
    sa = [sb("sa%d" % i, [128, 256], F32) for i in range(2)]
    hid = [sb("hid%d" % i, [128, 256], BF16) for i in range(2)]
    hidT = [sb("hidT%d" % i, [128, 2, 128], BF16) for i in range(2)]
    sm = sb("sm", [128, 32], F32)
    st6 = sb("st6", [128, 2, 6], F32)
    st6b = sb("st6b", [128, 1, 12], F32)
    rt = sb("rt", [128, 92], F32)
    gu1f = gu[1][:].rearrange("p k f -> p (k f)").bitcast(F32)
    OTsb = gu1f[:, 0:1024].rearrange("p (g c) -> p g c", g=2)
    hT32 = gu1f[:, 1024:2048].rearrange("p (k t) -> p k t", k=8)

    ps = [stack.enter_context(nc.psum_tensor("ps%d" % i, [128, 512], F32)) for i in range(8)]

    def psb(i):
        return ps[i].bitcast(BF16) if hasattr(ps[i], "bitcast") else None

    MX, MN, LO, WD, MID, CNT, GE, KT, RSTD, NMR, TMP, NMID, CNT2, KT2, BB, NMID2, GE2 = range(17)

    def smc(i):
        return sm[:, i:i + 1]

    op = P.op
    op("pool", "iota", iot_i[:], w=["iot_i"], pattern=[[1, 128]], base=0, channel_multiplier=-1)
    op("pool", "tensor_copy", r=["iot_i"], w=["iot_f"], out=iot_f[:], in_=iot_i[:])
    op("pool", "tensor_scalar", r=["iot_f"], w=["ident32"], out=ident32[:], in0=iot_f[:], scalar1=0.0,
       scalar2=None, op0=ALU.is_equal)
    for j in range(4):
        op("pool", "tensor_copy", r=["ident32"], w=["I4"], out=I4[:, j, :], in_=ident32[:])
    op("pool", "tensor_scalar", r=["iot_f"], w=["cbias"], out=cbias[:], in0=iot_f[:], scalar1=0.0,
       scalar2=-1e30, op0=ALU.is_gt, op1=ALU.mult)
    op("pool", "tensor_scalar", r=["iot_f"], w=["iop1"], out=iop1[:], in0=iot_f[:, 0:1], scalar1=-1.0,
       scalar2=1.0, op0=ALU.mult, op1=ALU.add)
    op("pool", "tensor_scalar", r=["iot_f", "iop1"], w=["pa"], out=pa[:, 0:16], in0=iot_f[:, 0:16],
       scalar1=iop1[:, 0:1], scalar2=None, op0=ALU.add)
    for g, win in enumerate(POOL_WINDOWS):
        op("dve", "tensor_scalar", r=["pa"], w=["pb"], out=pb[:, 0:16], in0=pa[:, 0:16], scalar1=float(win),
           scalar2=1.0 / win, op0=ALU.min, op1=ALU.mult)
        op("dve", "reciprocal", r=["pb"], w=["mrc"], out=mrc[:, g, :], in_=pb[:, 0:16])
    op("pool", "memset", negh[:], -0.5, w=["negh"])
    for j in range(n_bisect + 1):
        op("pool", "memset", CV[:, j:j + 1], 0.5 ** j, w=["CV"])
    op("pool", "memset", kTz[:], 0.0, w=["kTz"])
    op("pool", "memset", qiTz[:], 0.0, w=["qiTz"])
    op("pool", "memset", Vx[:], 1.0, w=["Vx"])
    for g in range(4):
        op("pool", "memset", ug[g][:], 0.0, w=["ug%d" % g])

    stg = [SC[0][:, 0:2048], SC[0][:, 2048:4096]]
    tmpb = [MBs[0][:, 0:2048], MBs[0][:, 2048:4096]]
    wfm_v = wfm_bf.rearrange("b p k c -> p b k c")
    last = []
    for kc in range(8):
        sl = kc % 2
        P.dma("sync", "stg%d" % sl, stg[sl][:, 0:NCOLS], win_d[kc * 128:(kc + 1) * 128, :], w=["stg%d" % sl])
        if kc % 2 == 0:
            op("act", "activation", r=["stg%d" % sl], w=["tmpb%d" % sl], out=tmpb[sl][:, 0:NCOLS],
               in_=stg[sl][:, 0:NCOLS], func=AF.Copy)
        else:
            op("dve", "tensor_copy", r=["stg%d" % sl], w=["tmpb%d" % sl], out=tmpb[sl][:, 0:NCOLS],
               in_=stg[sl][:, 0:NCOLS])
        P.dma("sync", "wst%d" % sl, wfm_v[:, :, kc, :], tmpb[sl][:, 0:NC_FM].rearrange("p (b c) -> p b c", c=128),
              r=["tmpb%d" % sl], w=["winbf%d" % sl])
        P.dma("sync", "wst%d" % sl, wtm_bf[:, kc, :], tmpb[sl][:, NC_FM:NCOLS], r=["tmpb%d" % sl],
              w=["winbf%d" % sl])
    for kc in range(8):
        sl = kc % 2
        P.dma("sync", "stg%d" % sl, stg[sl][:, 0:D], wout_d[kc * 128:(kc + 1) * 128, :], w=["stg%d" % sl])
        if kc % 2 == 0:
            tk = op("act", "activation", r=["stg%d" % sl], w=["Wout"], out=Wout[:, kc, :], in_=stg[sl][:, 0:D],
                    func=AF.Copy)
        else:
            tk = op("dve", "tensor_copy", r=["stg%d" % sl], w=["Wout"], out=Wout[:, kc, :], in_=stg[sl][:, 0:D])
        last.append(tk)
    P.readers.setdefault("sc0", []).extend(last[-2:])
    for sl in range(2):
        P.readers.setdefault("MB0", []).append(P.lastw["winbf%d" % sl])
        P.readers.setdefault("MBh0", []).append(P.lastw["winbf%d" % sl])
    P.dma("sync", "wtm", Wtm[:], wtm_bf, r=["winbf0", "winbf1"], w=["Wtm"])
    for g in range(4):
        P.dma("pool", "setup_p", wpool[:, g, :], wpool_d[g], w=["wpool"])
        P.dma("sync", "setup_s", psc[:, g:g + 1], psc_d[g * 128:(g + 1) * 128].unsqueeze(1), w=["psc"])
    for i in range(4):
        P.dma("sync", "setup_s", lnp[:, i, :], lnp_d[i].partition_broadcast(128), w=["lnp"])
    P.dma("sync", "setup_s", wr[:], wr_d.rearrange("(k p) c -> p k c", p=128), w=["wr"])
    tok_setup = P.dma("sync", "setup_s", br[:], br_d.partition_broadcast(128), w=["br"])
    for nme in ("psc", "lnp", "wr", "br"):
        P.lastw[nme] = tok_setup
    P.lastw["wpool"] = ("d", "setup_p", P.dcnt["setup_p"])
    for g, win in enumerate(POOL_WINDOWS):
        op("dve", "tensor_scalar", r=["psc"], w=["psc"], out=psc[:, g:g + 1], in0=psc[:, g:g + 1],
           scalar1=1.0 / win, scalar2=None, op0=ALU.mult)

    xT_v = xT_d.rearrange("(k p) t -> p k t", p=128)

    def load_xT(pr_):
        nm = "xTt%d" % (pr_ % 2)
        P.dma("pool", nm, xTt[pr_ % 2][:], xT_v[:, :, pr_ * 256:pr_ * 256 + 256], w=[nm])

    load_xT(0)
    if npairs > 1:
        load_xT(1)
    def wcast(ex):
        for hlf in range(2):
            P.dma("pool", "wc%d" % ex, wgu_bf[ex, hlf * 512:(hlf + 1) * 512, :],
                  wgu_d[ex, hlf * 512:(hlf + 1) * 512, :], w=["wbf%d" % ex])
        P.dma("pool", "wc%d" % ex, wd_bf[ex], wd_d[ex], w=["wbf%d" % ex])
        P.lastw["wbf%d" % ex] = ("d", "wc%d" % ex, P.dcnt["wc%d" % ex])

    for ex in range(4):
        wcast(ex)

    def load_gu(ex):
        s_ = ex % 2
        P.dma("sync", "gu%d" % s_, gu[s_][:], wgu_bf[ex].rearrange("(k p) f -> p k f", p=128),
              r=["wbf%d" % ex], w=["gu%d" % s_])

    def load_dd(ex):
        s_ = ex % 2
        P.dma("sync", "dd%d" % s_, dd[s_][:], wd_bf[ex].rearrange("(k p) d -> p k d", p=128),
              r=["wbf%d" % ex], w=["dd%d" % s_])

    def layer_norm(zt, zn, gi, bi):
        for dh in range(2):
            op("dve", "bn_stats", r=[zn], w=["st6"], out=st6[:, dh, :], in_=zt[:, dh * 512:(dh + 1) * 512])
        op("dve", "bn_aggr", r=["st6"], w=["sm"], out=sm[:, 24:26], in_=st6[:].rearrange("p a b -> p (a b)"))
        op("dve", "tensor_scalar", r=["sm"], w=["sm"], out=smc(TMP), in0=sm[:, 25:26], scalar1=LN_EPS,
           scalar2=None, op0=ALU.add)
        op("pool", "tensor_tensor", r=["sm", "negh"], w=["sm2"], out=smc(RSTD), in0=smc(TMP), in1=negh[:],
           op=ALU.pow)
        op("dve", "scalar_tensor_tensor", r=["sm", "sm2"], w=["sm3"], out=smc(NMR), in0=sm[:, 24:25], scalar=-1.0,
           in1=smc(RSTD), op0=ALU.mult, op1=ALU.mult)
        op("act", "activation", r=[zn, "sm2", "sm3"], w=[zn], out=zt, in_=zt, func=AF.Identity, bias=smc(NMR),
           scale=smc(RSTD))
        op("dve", "tensor_tensor", r=[zn, "lnp"], w=[zn], out=zt, in0=zt, in1=lnp[:, gi, :], op=ALU.mult)
        op("pool", "tensor_tensor", r=[zn, "lnp"], w=[zn], out=zt, in0=zt, in1=lnp[:, bi, :], op=ALU.add)

    wb_ctr = [0]

    def proj_gen(pr, cbs, tokmajor):
        t0 = pr * 256
        xt = xTt[pr % 2]
        xn = "xTt%d" % (pr % 2)
        bi = 0
        slots = []

        def issue(cb_):
            wi_ = wb_ctr[0] % 4
            wb_ctr[0] += 1
            P.dma("sync", "Wb%d" % wi_, Wb[wi_][:], wfm_bf[cb_], r=["winbf0", "winbf1"], w=["Wb%d" % wi_])
            slots.append(wi_)

        for cb_ in cbs[:3]:
            issue(cb_)
        for ci_, cb in enumerate(cbs):
            if ci_ + 3 < len(cbs):
                issue(cbs[ci_ + 3])
            wi_ = slots[ci_]
            wn = "Wb%d" % wi_
            bank = bi % 2
            bi += 1
            bn = "ps%d" % bank
            pz = ps[bank]
            for kc in range(8):
                op("pe", "matmul", pz[:, 0:256], r=[wn, xn], w=[bn], lhsT=Wb[wi_][:, kc, :],
                   rhs=xt[:, kc, :], start=(kc == 0), stop=(kc == 7))
            if cb < 4:
                op("act", "activation", r=[bn], w=["qTt"], out=qTt[:, cb, :], in_=pz[:, 0:256], func=AF.Copy)
            elif cb == 4:
                op("act", "activation", r=[bn], w=["kTz"], out=kTz[0:64, 0, t0:t0 + 256], in_=pz[0:64, 0:256],
                   func=AF.Copy)
                op("act", "activation", r=[bn], w=["kTz"], out=kTz[64:128, 1, t0:t0 + 256], in_=pz[64:128, 0:256],
                   func=AF.Copy)
            elif cb < 9:
                g = cb - 5
                op("act", "activation", r=[bn], w=["ug%d" % g], out=ug[g][:, 16:272], in_=pz[:, 0:256],
                   func=AF.Copy)
            elif cb < 13:
                j = cb - 9
                op("act", "activation", r=[bn], w=["qiTz"], out=qiTz[0:64, j, :], in_=pz[0:64, 0:256], func=AF.Copy)
                op("act", "activation", r=[bn], w=["qiTz"], out=qiTz[64:128, 4 + j, :], in_=pz[64:128, 0:256],
                   func=AF.Copy)
            else:
                op("act", "activation", r=[bn], w=["kiT"], out=kiT[:, t0:t0 + 256], in_=pz[:, 0:256], func=AF.Copy)
            yield cb
        if tokmajor:
            for st in range(2):
                blk = pr * 2 + st
                bank = bi % 2
                bi += 1
                bn = "ps%d" % bank
                pz = ps[bank]
                for kc in range(8):
                    op("pe", "matmul", pz[:, 0:NC_TM], r=["Wtm", xn], w=[bn],
                       lhsT=xt[:, kc, st * 128:(st + 1) * 128], rhs=Wtm[:, kc, :], start=(kc == 0),
                       stop=(kc == 7))
                op("act", "activation", r=[bn], w=["Vx"], out=Vx[:, blk, :, 0:64],
                   in_=pz[:, 0:128].rearrange("p (g c) -> p g c", g=2), func=AF.Copy)
                op("act", "activation", r=[bn], w=["wI"], out=wI[:, blk, :], in_=pz[:, 128:136], func=AF.Copy)
                yield 100 + st

    def proj_blocks(pr, cbs, tokmajor):
        for _ in proj_gen(pr, cbs, tokmajor):
            pass

    def chain(*gens):
        for g_ in gens:
            if g_ is not None:
                for v_ in g_:
                    yield v_

    def pooling_gen(pr):
        for g, win in enumerate(POOL_WINDOWS):
            un = "ug%d" % g
            u = ug[g]
            cur, curn = u, un
            sh = 1
            for lvl in range(g + 1):
                dst, dstn = (pa, "pa") if lvl % 2 == 0 else (pb, "pb")
                op("pool", "tensor_tensor", r=[curn], w=[dstn], out=dst[:, sh:272], in0=cur[:, sh:272],
                   in1=cur[:, 0:272 - sh], op=ALU.add)
                cur, curn = dst, dstn
                sh *= 2
            if pr == 0:
                op("pool", "tensor_tensor", r=[curn, "mrc"], w=[curn], out=cur[:, 16:32], in0=cur[:, 16:32],
                   in1=mrc[:, g, :], op=ALU.mult)
            yield 0
            dn = "dTg%d" % (g % 2)
            dt_ = dTg[g % 2]
            op("dve", "scalar_tensor_tensor", r=[un, curn], w=[dn], out=dt_[:], in0=u[:, 16:272],
               scalar=-float(win), in1=cur[:, 16:272], op0=ALU.mult, op1=ALU.add)
            yield 1
            op("pool", "tensor_copy", r=[un], w=[un], out=u[:, 0:16], in_=u[:, 256:272])
            bank = 7
            bn = "ps%d" % bank
            op("pe", "matmul", ps[bank][:, 0:256], r=["wpool", dn], w=[bn], lhsT=wpool[:, g, :], rhs=dt_[:],
               start=True, stop=True)
            op("act", "activation", r=[bn, "psc"], w=["poolTt"], out=poolTt[:, g, :], in_=ps[bank][:, 0:256],
               func=AF.Identity, scale=psc[:, g:g + 1])
            yield 3

    def acc_gen(qb):
        qq = qb % 2
        nk = (qb + 1) * 128
        tq = slice(qq * 128, (qq + 1) * 128)
        scores = SC[qb % 2]
        scn = "sc%d" % (qb % 2)
        nch = (nk + 511) // 512
        tiles = [(c, h) for c in range(nch) for h in range(8)]

        def f_mm(i):
            c, h = tiles[i]
            k0 = c * 512
            wdt = min(512, nk - k0)
            bank = i % 2
            op("pe", "matmul", ps[bank][:, 0:wdt], r=["qiTz", "kiT"], w=["ps%d" % bank], lhsT=qiTz[:, h, tq],
               rhs=kiT[:, k0:k0 + wdt], start=True, stop=True)

        def f_relu(i):
            c, h = tiles[i]
            k0 = c * 512
            wdt = min(512, nk - k0)
            bank = i % 2
            op("act", "activation", r=["ps%d" % bank], w=["R%d" % (i % 3)], out=Rb[i % 3][:, 0:wdt],
               in_=ps[bank][:, 0:wdt], func=AF.Relu)

        def back(i):
            c, h = tiles[i]
            k0 = c * 512
            wdt = min(512, nk - k0)
            rb = Rb[i % 3]
            rn = "R%d" % (i % 3)
            if h == 0:
                op("dve", "tensor_scalar", r=[rn, "wI"], w=[scn], out=scores[:, k0:k0 + wdt],
                   in0=rb[:, 0:wdt], scalar1=wI[:, qb, h:h + 1], scalar2=None, op0=ALU.mult)
            else:
                op("dve", "scalar_tensor_tensor", r=[rn, "wI", scn], w=[scn],
                   out=scores[:, k0:k0 + wdt], in0=rb[:, 0:wdt], scalar=wI[:, qb, h:h + 1],
                   in1=scores[:, k0:k0 + wdt], op0=ALU.mult, op1=ALU.add)

        nt = len(tiles)
        for i in range(nt + 3):
            if i < nt:
                f_mm(i)
            if 0 <= i - 1 < nt:
                f_relu(i - 1)
            if 0 <= i - 3 < nt:
                back(i - 3)
            yield i

    def bis_gen(qb):
        nk = (qb + 1) * 128
        scores = SC[qb % 2]
        scn = "sc%d" % (qb % 2)
        MB = MBs[qb % 2]
        mbn, mbh = "MB%d" % (qb % 2), "MBh%d" % (qb % 2)
        sc = scores[:, 0:nk]
        SM = dict(r=["sm"], w=["sm"])
        op("dve", "tensor_reduce", r=[scn], w=["sm"], out=smc(MX), in_=sc, axis=AX.X, op=ALU.max)
        op("dve", "tensor_reduce", r=[scn], w=["sm"], out=smc(MN), in_=sc, axis=AX.X, op=ALU.min)
        dg = scores[:, qb * 128:(qb + 1) * 128]
        op("dve", "tensor_tensor", r=[scn, "cbias"], w=[scn], out=dg, in0=dg, in1=cbias[:], op=ALU.add)
        op("dve", "tensor_scalar", r=["iop1"], w=["sm"], out=smc(KT), in0=iop1[:], scalar1=float(qb * 128),
           scalar2=float(TOPK), op0=ALU.add, op1=ALU.min)
        op("dve", "tensor_tensor", out=smc(WD), in0=smc(MX), in1=smc(MN), op=ALU.subtract, **SM)
        op("dve", "tensor_scalar", out=smc(WD), in0=smc(WD), scalar1=1.0 + 1.0 / 1024, scalar2=1e-12,
           op0=ALU.mult, op1=ALU.add, **SM)
        op("dve", "tensor_scalar", r=["sm", "CV"], w=["WDS"], out=WDS[:, 0:n_bisect + 1], in0=CV[:, 0:n_bisect + 1],
           scalar1=smc(WD), scalar2=None, op0=ALU.mult)
        op("dve", "tensor_tensor", r=["sm", "WDS"], w=["sm"], out=smc(MID), in0=smc(MN), in1=WDS[:, 1:2], op=ALU.add)
        yield 0
        for itb in range(1, n_bisect + 1):
            op("dve", "tensor_scalar", r=[scn, "sm"], w=[mbn, "sm"], out=MB[:, 0:nk], in0=sc,
               scalar1=smc(MID), scalar2=0.0, op0=ALU.is_ge, op1=ALU.add, accum_out=smc(CNT))
            op("dve", "tensor_scalar", out=smc(GE), in0=smc(CNT), scalar1=smc(KT), scalar2=0.5, op0=ALU.is_ge,
               op1=ALU.subtract, **SM)
            if itb < n_bisect:
                op("dve", "scalar_tensor_tensor", r=["sm", "WDS"], w=["sm"], out=smc(MID), in0=smc(GE),
                   scalar=WDS[:, itb:itb + 1], in1=smc(MID), op0=ALU.mult, op1=ALU.add)
            yield itb
        op("dve", "tensor_scalar", r=["sm", "WDS"], w=["sm"], out=smc(TMP), in0=smc(GE), scalar1=-1.0,
           scalar2=WDS[:, n_bisect:n_bisect + 1], op0=ALU.add, op1=ALU.mult)
        op("dve", "tensor_tensor", out=smc(LO), in0=smc(TMP), in1=smc(MID), op=ALU.add, **SM)
        op("dve", "tensor_scalar", r=[scn, "sm"], w=[mbn, mbh], out=MB[:, 0:nk], in0=sc, scalar1=smc(LO),
           scalar2=NEG_MASK, op0=ALU.is_lt, op1=ALU.mult)
        yield 99

    def bis_gen_act(qb, nh=8):
        nk = (qb + 1) * 128
        scores = SC[qb % 2]
        scn = "sc%d" % (qb % 2)
        MB = MBs[qb % 2]
        mbn, mbh = "MB%d" % (qb % 2), "MBh%d" % (qb % 2)
        sc = scores[:, 0:nk]
        SM = dict(r=["sm"], w=["sm"])
        op("dve", "tensor_reduce", r=[scn], w=["sm"], out=smc(MX), in_=sc, axis=AX.X, op=ALU.max)
        op("dve", "tensor_reduce", r=[scn], w=["sm"], out=smc(MN), in_=sc, axis=AX.X, op=ALU.min)
        dg = scores[:, qb * 128:(qb + 1) * 128]
        op("dve", "tensor_tensor", r=[scn, "cbias"], w=[scn], out=dg, in0=dg, in1=cbias[:], op=ALU.add)
        op("dve", "tensor_scalar", r=["iop1"], w=["sm"], out=smc(KT), in0=iop1[:], scalar1=float(qb * 128),
           scalar2=float(TOPK), op0=ALU.add, op1=ALU.min)
        op("dve", "tensor_scalar", r=["sm"], w=["smb"], out=smc(BB), in0=smc(KT), scalar1=-2.0,
           scalar2=float(nk) + 0.5, op0=ALU.mult, op1=ALU.add)
        op("dve", "tensor_tensor", out=smc(WD), in0=smc(MX), in1=smc(MN), op=ALU.subtract, **SM)
        op("dve", "tensor_scalar", out=smc(WD), in0=smc(WD), scalar1=1.0 + 1.0 / 1024, scalar2=1e-12,
           op0=ALU.mult, op1=ALU.add, **SM)
        op("dve", "tensor_scalar", r=["sm", "CV"], w=["WDS"], out=WDS[:, 0:n_bisect + 1], in0=CV[:, 0:n_bisect + 1],
           scalar1=smc(WD), scalar2=None, op0=ALU.mult)
        op("dve", "tensor_scalar", r=["sm", "CV"], w=["NWDS"], out=NWDS[:, 0:n_bisect + 1],
           in0=CV[:, 0:n_bisect + 1], scalar1=smc(WD), scalar2=-1.0, op0=ALU.mult, op1=ALU.mult)
        op("dve", "tensor_tensor", r=["sm", "WDS"], w=["sm"], out=smc(MID), in0=smc(MN), in1=WDS[:, 1:2], op=ALU.add)
        nm = [smc(NMID), smc(NMID2)]
        op("dve", "tensor_scalar", r=["sm"], w=["nmid1"], out=nm[1], in0=smc(MID), scalar1=-1.0, scalar2=None,
           op0=ALU.mult)
        yield 0
        for itb in range(1, n_bisect + 1):
            cur, nxt = itb % 2, (itb + 1) % 2
            if itb % 2 == 1:
                op("dve", "tensor_scalar", r=[scn, "smmid"] if itb > 1 else [scn, "sm"], w=[mbn, "sm"],
                   out=MB[:, 0:nk], in0=sc, scalar1=smc(MID), scalar2=0.0, op0=ALU.is_ge, op1=ALU.add,
                   accum_out=smc(CNT))
                op("dve", "tensor_scalar", out=smc(GE), in0=smc(CNT), scalar1=smc(KT), scalar2=0.5, op0=ALU.is_ge,
                   op1=ALU.subtract, **SM)
                op("dve", "scalar_tensor_tensor", r=["sm", "NWDS", "nmid%d" % cur], w=["nmid%d" % nxt],
                   out=nm[nxt], in0=smc(GE), scalar=NWDS[:, itb:itb + 1], in1=nm[cur], op0=ALU.mult, op1=ALU.add)
            else:
                op("act", "activation", r=[scn, "nmid%d" % cur], w=[mbh, "smc2"], out=MB[:, 0:nk], in_=sc,
                   func=AF.Sign, bias=nm[cur], scale=1.0, accum_out=smc(CNT2))
                op("act", "activation", r=["smc2", "smb"], w=["smg"], out=smc(GE2), in_=smc(CNT2), func=AF.Sign,
                   bias=smc(BB), scale=1.0)
                if itb < n_bisect:
                    op("act", "activation", r=["smg", "NWDS", "nmid%d" % cur], w=["nmid%d" % nxt], out=nm[nxt],
                       in_=smc(GE2), func=AF.Identity, bias=nm[cur], scale=NWDS[:, itb + 1:itb + 2])
                    op("act", "activation", r=["nmid%d" % nxt], w=["smmid"], out=smc(MID), in_=nm[nxt],
                       func=AF.Identity, scale=-1.0)
            yield itb
        assert n_bisect % 2 == 0
        cur = n_bisect % 2
        op("dve", "tensor_scalar", r=["smg"], w=["sm"], out=smc(TMP), in0=smc(GE2), scalar1=0.5, scalar2=-1.0,
           op0=ALU.mult, op1=ALU.add)
        op("dve", "scalar_tensor_tensor", r=["sm", "WDS", "nmid%d" % cur], w=["sm"], out=smc(LO), in0=smc(TMP),
           scalar=WDS[:, n_bisect:n_bisect + 1], in1=nm[cur], op0=ALU.mult, op1=ALU.subtract)
        op("dve", "tensor_scalar", r=[scn, "sm", mbh], w=[mbn, mbh], out=MB[:, 0:nk], in0=sc, scalar1=smc(LO),
           scalar2=NEG_MASK, op0=ALU.is_lt, op1=ALU.mult)
        yield 99

    def attn_units(qb):
        qq = qb % 2
        tq = slice(qq * 128, (qq + 1) * 128)
        MB = MBs[qb % 2]
        mbn, mbh = "MB%d" % (qb % 2), "MBh%d" % (qb % 2)
        units = [(sb_, g) for sb_ in range(qb + 1) for g in range(2)]

        def qkmm(i):
            sb_, g = units[i]
            bank = 2 + (i % 2)
            bn = "ps%d" % bank
            op("pe", "matmul", ps[bank][:, :], r=["kTz", "qTt"], w=[bn], lhsT=kTz[:, g, sb_ * 128:(sb_ + 1) * 128],
               rhs=qTt[:, :, tq], start=True, stop=False)
            op("pe", "matmul", ps[bank][:, :], r=[mbn, mbh, "I4"], w=[bn], lhsT=MB[:, sb_ * 128:(sb_ + 1) * 128],
               rhs=I4[:, :, :], start=False, stop=True)

        def ex(i):
            bank = 2 + (i % 2)
            op("act", "activation", r=["ps%d" % bank], w=["PT%d" % (i % 3)], out=PT[i % 3][:], in_=ps[bank][:, :],
               func=AF.Exp, scale=0.125)

        def pv(i):
            sb_, g = units[i]
            bank = 4 + g
            op("pe", "matmul", ps[bank][0:65, :], r=["Vx", "PT%d" % (i % 3)], w=["ps%d" % bank],
               lhsT=Vx[:, sb_, g, :], rhs=PT[i % 3][:], start=(sb_ == 0), stop=(sb_ == qb))

        nu = len(units)
        qkmm(0)
        if nu > 1:
            qkmm(1)
        ex(0)
        for i in range(nu):
            if i + 2 < nu:
                qkmm(i + 2)
            if i + 1 < nu:
                ex(i + 1)
            pv(i)
            yield i
        for g in range(2):
            op("act", "activation", r=["ps%d" % (4 + g)], w=["OTs"], out=OTs[0:65, g, :],
               in_=ps[4 + g][0:65, :], func=AF.Copy)
        yield nu

    def tail_gen(qb):
        qq = qb % 2
        tq = slice(qq * 128, (qq + 1) * 128)
        zn = "z%d" % qq
        zt = z[:, qq, :]
        for g in range(2):
            bank = 6 + g
            for j in range(4):
                op("pe", "transpose", ps[bank][:, j * 66:j * 66 + 65], OTs[0:65, g, j * 128:(j + 1) * 128],
                   ident32[0:65, 0:65], r=["OTs", "ident32"], w=["ps%d" % bank])
        yield 1
        for g in range(2):
            bank = 6 + g
            pv_ = ps[bank][:, 0:264].rearrange("p (j c) -> p j c", j=4)
            op("dve", "reciprocal", r=["ps%d" % bank], w=["rt"], out=rt[:, g * 4:(g + 1) * 4], in_=pv_[:, :, 64])
            op("dve", "tensor_tensor", r=["ps%d" % bank, "rt"], w=["Otok"], out=Otok[:, g * 4:(g + 1) * 4, :],
               in0=pv_[:, :, 0:64], in1=rt[:, g * 4:(g + 1) * 4].unsqueeze(2).to_broadcast([128, 4, 64]),
               op=ALU.mult)
        yield 2
        p6 = ps[6].bitcast(BF16)
        for c in range(4):
            op("pe", "transpose", p6[:, c * 128:(c + 1) * 128],
               Otok[:, 2 * c:2 * c + 2, :].rearrange("p a b -> p (a b)"), I4[:, 0, :],
               r=["Otok", "I4"], w=["ps6"])
        yield 3
        op("act", "activation", r=["ps6"], w=["catT"], out=catT[:].rearrange("p a b -> p (a b)"),
           in_=p6[:, 0:512], func=AF.Copy)
        yield 4
        for dh in range(2):
            bank = 6 + dh
            for c in range(8):
                lt = catT[:, c, :] if c < 4 else poolTt[:, c - 4, tq]
                op("pe", "matmul", ps[bank][:, :], r=["catT", "poolTt", "Wout"], w=["ps%d" % bank], lhsT=lt,
                   rhs=Wout[:, c, dh * 512:(dh + 1) * 512], start=(c == 0), stop=(c == 7))
        yield 5
        for dh in range(2):
            zs = z[:, qq, dh * 512:(dh + 1) * 512]
            op("dve", "scalar_tensor_tensor", r=[zn, "ps%d" % (6 + dh)], w=[zn], out=zs, in0=zs, scalar=ALPHA,
               in1=ps[6 + dh][:, :], op0=ALU.mult, op1=ALU.add)
        for dh in range(2):
            op("dve", "bn_stats", r=[zn], w=["st6"], out=st6[:, dh, :], in_=zt[:, dh * 512:(dh + 1) * 512])
        op("dve", "bn_aggr", r=["st6"], w=["sm"], out=sm[:, 24:26], in_=st6[:].rearrange("p a b -> p (a b)"))
        op("dve", "tensor_scalar", r=["sm"], w=["sm"], out=smc(TMP), in0=sm[:, 25:26], scalar1=LN_EPS,
           scalar2=None, op0=ALU.add)
        yield 6
        op("pool", "tensor_tensor", r=["sm", "negh"], w=["sm2"], out=smc(RSTD), in0=smc(TMP), in1=negh[:],
           op=ALU.pow)
        yield 7
        op("dve", "scalar_tensor_tensor", r=["sm", "sm2"], w=["sm3"], out=smc(NMR), in0=sm[:, 24:25], scalar=-1.0,
           in1=smc(RSTD), op0=ALU.mult, op1=ALU.mult)
        yield 8
        op("act", "activation", r=[zn, "sm2", "sm3"], w=[zn], out=zt, in_=zt, func=AF.Identity, bias=smc(NMR),
           scale=smc(RSTD))
        yield 9
        op("dve", "tensor_tensor", r=[zn, "lnp"], w=[zn], out=zt, in0=zt, in1=lnp[:, 0, :], op=ALU.mult)
        op("dve", "tensor_tensor", r=[zn, "lnp"], w=[zn], out=zt, in0=zt, in1=lnp[:, 1, :], op=ALU.add)
        yield 10
        for half in range(2):
            bank = 6 + half
            for k4 in range(4):
                dc = half * 4 + k4
                op("pe", "transpose", ps[bank][:, k4 * 128:(k4 + 1) * 128], z[:, qq, dc * 128:(dc + 1) * 128],
                   ident32[:], r=[zn, "ident32"], w=["ps%d" % bank])
        yield 11
        for half in range(2):
            pview = ps[6 + half][:, :].rearrange("p (k t) -> p k t", k=4)
            op("act", "activation", r=["ps%d" % (6 + half)], w=["gu1"], out=hT32[:, half * 4:(half + 1) * 4, :],
               in_=pview, func=AF.Copy)
        yield 12
        for half in range(2):
            op("dve", "tensor_copy", r=["gu1"], w=["hTb"], out=hTb[:, half * 4:(half + 1) * 4, tq],
               in_=hT32[:, half * 4:(half + 1) * 4, :])
        for dc in range(8):
            op("pe", "matmul", ps[6][:, 0:20], r=["gu1", "wr"], w=["ps6"], lhsT=hT32[:, dc, :],
               rhs=wr[:, dc, :], start=(dc == 0), stop=(dc == 7))
        yield 13
        LG = rt[:, 16:36]
        GL = rt[:, 16:20]
        EL = rt[:, 20:36].rearrange("p (g e) -> p g e", g=4)
        GMAX, GNEG, GSUM, GW = rt[:, 36:37], rt[:, 37:38], rt[:, 38:39], rt[:, 39:40]
        GOH = rt[:, 40:44]
        TM16 = rt[:, 44:60]
        ES = rt[:, 60:64]
        M1, M2, DDIF, ED, W1, W2 = (rt[:, 64 + i:65 + i] for i in range(6))
        OH1, ES2, OH2, IG = rt[:, 72:76], rt[:, 76:80], rt[:, 80:84], rt[:, 84:88]
        GEX = rt[:, 88:92]
        RT = dict(r=["rt"], w=["rt"])
        op("dve", "tensor_tensor", r=["ps6", "br"], w=["rt"], out=LG, in0=ps[6][:, 0:20], in1=br[:], op=ALU.add)
        op("dve", "tensor_reduce", out=GMAX, in_=GL, axis=AX.X, op=ALU.max, **RT)
        op("dve", "tensor_scalar", out=GNEG, in0=GMAX, scalar1=-1.0, scalar2=None, op0=ALU.mult, **RT)
        op("dve", "tensor_scalar", out=GOH, in0=GL, scalar1=GMAX, scalar2=None, op0=ALU.is_equal, **RT)
        op("dve", "tensor_tensor", out=TM16.rearrange("p (g e) -> p g e", g=4), in0=EL,
           in1=GOH.unsqueeze(2).to_broadcast([128, 4, 4]), op=ALU.mult, **RT)
        op("dve", "tensor_reduce", out=ES, in_=TM16.rearrange("p (g e) -> p e g", g=4), axis=AX.X, op=ALU.add,
           **RT)
        op("dve", "tensor_reduce", out=M1, in_=ES, axis=AX.X, op=ALU.max, **RT)
        op("dve", "tensor_scalar", out=OH1, in0=ES, scalar1=M1, scalar2=None, op0=ALU.is_equal, **RT)
        op("dve", "scalar_tensor_tensor", out=ES2, in0=OH1, scalar=-1e30, in1=ES, op0=ALU.mult, op1=ALU.add,
           **RT)
        op("dve", "tensor_reduce", out=M2, in_=ES2, axis=AX.X, op=ALU.max, **RT)
        op("dve", "tensor_scalar", out=OH2, in0=ES2, scalar1=M2, scalar2=None, op0=ALU.is_equal, **RT)
        op("dve", "tensor_tensor", out=DDIF, in0=M2, in1=M1, op=ALU.subtract, **RT)
        yield 14
        op("act", "activation", r=["rt"], w=["rt2"], out=GEX, in_=GL, func=AF.Exp, bias=GNEG, scale=1.0,
           accum_out=GSUM)
        op("act", "activation", r=["rt"], w=["rt3"], out=ED, in_=DDIF, func=AF.Exp)
        yield 15
        op("dve", "reciprocal", r=["rt2"], w=["rt"], out=GW, in_=GSUM)
        op("dve", "tensor_scalar", r=["rt3"], w=["rt"], out=W1, in0=ED, scalar1=1.0, scalar2=None, op0=ALU.add)
        op("dve", "reciprocal", out=W1, in_=W1, **RT)
        op("dve", "tensor_tensor", out=W1, in0=W1, in1=GW, op=ALU.mult, **RT)
        op("dve", "tensor_tensor", r=["rt", "rt3"], w=["rt"], out=W2, in0=W1, in1=ED, op=ALU.mult)
        op("dve", "tensor_scalar", out=IG, in0=OH1, scalar1=W1, scalar2=None, op0=ALU.mult, **RT)
        op("dve", "scalar_tensor_tensor", out=IG, in0=OH2, scalar=W2, in1=IG, op0=ALU.mult, op1=ALU.add, **RT)
        op("dve", "tensor_tensor", r=["rt"], w=["gate"], out=gate[:, qq, :].rearrange("p (g e) -> p g e", g=4),
           in0=GOH.unsqueeze(2).to_broadcast([128, 4, 4]), in1=IG.unsqueeze(1).to_broadcast([128, 4, 4]),
           op=ALU.mult)
        yield 16

    def moe_gen(pr):
        t0 = pr * 256
        unitsm = [(ex, st) for ex in range(N_EXP) for st in range(2)]
        nm_ = len(unitsm)
        load_gu(1)

        def m_ab_mm(i):
            ex, st = unitsm[i]
            s_ = ex % 2
            bank = 4 + (i % 2)
            bn = "ps%d" % bank
            for kc in range(8):
                op("pe", "matmul", ps[bank][:, :], r=["hTb", "gu%d" % s_], w=[bn],
                   lhsT=hTb[:, kc, st * 128:(st + 1) * 128], rhs=gu[s_][:, kc, :], start=(kc == 0), stop=(kc == 7))

        def m_ab_ew(i):
            ex, st = unitsm[i]
            bank = 4 + (i % 2)
            bn = "ps%d" % bank
            op("act", "activation", r=[bn], w=["sa%d" % (i % 2)], out=sa[i % 2][:], in_=ps[bank][:, 0:256],
               func=AF.Silu)
            op("dve", "scalar_tensor_tensor", r=[bn, "gate", "sa%d" % (i % 2)], w=["hid%d" % (i % 2)],
               out=hid[i % 2][:], in0=ps[bank][:, 256:512], scalar=gate[:, st, ex:ex + 1], in1=sa[i % 2][:],
               op0=ALU.mult, op1=ALU.mult)

        def m_t(i):
            bank = 6 + (i % 2)
            pb_ = ps[bank].bitcast(BF16)
            for fc in range(2):
                op("pe", "transpose", pb_[:, fc * 128:(fc + 1) * 128], hid[i % 2][:, fc * 128:(fc + 1) * 128],
                   I4[:, 0, :], r=["hid%d" % (i % 2), "I4"], w=["ps%d" % bank])
            op("act", "activation", r=["ps%d" % bank], w=["hidT%d" % (i % 2)],
               out=hidT[i % 2][:].rearrange("p a b -> p (a b)"), in_=pb_[:, 0:256], func=AF.Copy)

        def m_d(i):
            ex, st = unitsm[i]
            s_ = ex % 2
            for dh in range(2):
                bank = st * 2 + dh
                for fc in range(2):
                    op("pe", "matmul", ps[bank][:, :], r=["hidT%d" % (i % 2), "dd%d" % s_], w=["ps%d" % bank],
                       lhsT=hidT[i % 2][:, fc, :], rhs=dd[s_][:, fc, dh * 512:(dh + 1) * 512],
                       start=(ex == 0 and fc == 0), stop=(ex == N_EXP - 1 and fc == 1))

        for rnd in range(nm_ + 4):
            if rnd % 2 == 0 and 2 <= rnd < nm_:
                ex = rnd // 2
                if ex + 1 < N_EXP:
                    load_gu(ex + 1)
            if rnd < nm_:
                m_ab_mm(rnd)
            if 0 <= rnd - 1 < nm_:
                m_ab_ew(rnd - 1)
            if 0 <= rnd - 2 < nm_:
                m_t(rnd - 2)
            if 0 <= rnd - 3 < nm_:
                m_d(rnd - 3)
            if rnd % 2 == 0 and 2 <= rnd - 2 < nm_:
                ex = (rnd - 2) // 2
                if ex + 1 < N_EXP:
                    load_dd(ex + 1)
            yield rnd

    def moe_fin_gen(pr, zp):
        t0 = pr * 256
        MV, TMP2, RSTD2, NMR2 = sm[:, 28:30], sm[:, 27:28], sm[:, 30:31], sm[:, 31:32]
        for st in range(2):
            zn = "z%d" % st
            zt = z[:, st, :]
            if st == 0:
                for dh in range(2):
                    bank = dh
                    zs = z[:, 0, dh * 512:(dh + 1) * 512]
                    op("dve", "scalar_tensor_tensor", r=["z0", "ps%d" % bank], w=["z0"], out=zs, in0=zs,
                       scalar=ALPHA, in1=ps[bank][:, :], op0=ALU.mult, op1=ALU.add)
                for dh in range(2):
                    bank = 2 + dh
                    zs = z[:, 1, dh * 512:(dh + 1) * 512]
                    op("dve", "scalar_tensor_tensor", r=["z1", "ps%d" % bank], w=["z1"], out=zs, in0=zs,
                       scalar=ALPHA, in1=ps[bank][:, :], op0=ALU.mult, op1=ALU.add)
            for dh in range(2):
                op("dve", "bn_stats", r=[zn], w=["st6b"], out=st6b[:, 0, dh * 6:(dh + 1) * 6],
                   in_=zt[:, dh * 512:(dh + 1) * 512])
            op("dve", "bn_aggr", r=["st6b"], w=["smf"], out=MV, in_=st6b[:, 0, :])
            op("dve", "tensor_scalar", r=["smf"], w=["smf"], out=TMP2, in0=sm[:, 29:30], scalar1=LN_EPS,
               scalar2=None, op0=ALU.add)
            yield 0
            op("pool", "tensor_tensor", r=["smf", "negh"], w=["smf2"], out=RSTD2, in0=TMP2, in1=negh[:], op=ALU.pow)
            yield 1
            op("dve", "scalar_tensor_tensor", r=["smf", "smf2"], w=["smf3"], out=NMR2, in0=sm[:, 28:29], scalar=-1.0,
               in1=RSTD2, op0=ALU.mult, op1=ALU.mult)
            yield 2
            op("act", "activation", r=[zn, "smf2", "smf3"], w=[zn], out=zt, in_=zt, func=AF.Identity, bias=NMR2,
               scale=RSTD2)
            yield 3
            op("dve", "tensor_tensor", r=[zn, "lnp"], w=[zn], out=zt, in0=zt, in1=lnp[:, 2, :], op=ALU.mult)
            op("dve", "tensor_tensor", r=[zn, "lnp"], w=[zn], out=zt, in0=zt, in1=lnp[:, 3, :], op=ALU.add)
            yield 4
            P.dma("sync", "out%d" % st, out_d[t0 + st * 128:t0 + (st + 1) * 128, :], zt, r=[zn], w=["o%d" % st])
            if 0 <= zp < npairs:
                P.dma("sync", "z%d" % st, zt, x_d[zp * 256 + st * 128:zp * 256 + (st + 1) * 128, :], w=[zn])
            yield 5
        if 0 <= zp < npairs:
            load_gu(0); load_dd(0); load_dd(1)
        yield 6

    def idle(n):
        for _ in range(n):
            yield 0

    IDX_BLOCKS = [9, 10, 11, 12, 13]
    MIX_BLOCKS = [0, 1, 2, 3, 4, 5, 6, 7, 8]
    nblk = 2 * npairs

    def run_threads(threads):
        active = [t for t in threads if t[0] is not None]
        while active:
            maxc = max(t[1] for t in active)
            for jj in range(maxc):
                for t in list(active):
                    if t[1] > jj and t in active:
                        try:
                            next(t[0])
                        except StopIteration:
                            active.remove(t)

    def per(n, ticks=16):
        return max(1, -(-n // ticks))

    proj_blocks(0, IDX_BLOCKS, True)
    run_threads([[acc_gen(0), 1]])
    for k in range(nblk + 3):
        bg = None
        if k < nblk:
            bg = bis_gen(k) if (k % 2 == 0 and 0 <= k // 2 - 2 < npairs) else bis_gen_act(k)
        lg = tail_gen(k - 2) if 0 <= k - 2 < nblk else None
        if k % 2 == 1:
            p1, p0 = (k + 1) // 2, (k - 1) // 2
            pj = chain(proj_gen(p1, IDX_BLOCKS, True) if p1 < npairs else None,
                       proj_gen(p0, MIX_BLOCKS, False) if p0 < npairs else None)
            act_ = [[lg, 1], [bg, 1], [lg, 1], [pj, 6]]
            while True:
                live = False
                for t_ in act_:
                    if t_[0] is None:
                        continue
                    for _ in range(t_[1]):
                        try:
                            next(t_[0])
                            if t_[0] is pj:
                                live = True
                        except StopIteration:
                            if t_[0] is pj:
                                live = False
                            t_[0] = None if t_[0] is pj else t_[0]
                            break
                if not live:
                    break
            if p0 < npairs and p0 + 2 < npairs:
                load_xT(p0 + 2)
        else:
            j = k // 2 - 2
            zp = k // 2 - 1
            fg = None
            if 0 <= j < npairs:
                run_threads([[moe_gen(j), 2], [bg, 1]])
                bg = None
                fg = moe_fin_gen(j, zp)
                if lg is not None:
                    lg = chain(idle(4), lg)
            elif 0 <= zp < npairs:
                for qq in range(2):
                    P.dma("sync", "z%d" % qq, z[:, qq, :], x_d[zp * 256 + qq * 128:zp * 256 + (qq + 1) * 128, :],
                          w=["z%d" % qq])
                load_gu(0); load_dd(0); load_dd(1)
        ag = acc_gen(k + 1) if k + 1 < nblk else None
        tg = attn_units(k - 1) if 0 <= k - 1 < nblk else None
        nt_acc = 8 * (((k + 2) * 128 + 511) // 512) + 2 if ag is not None else 0
        pg = pooling_gen((k - 1) // 2) if (k % 2 == 1 and (k - 1) // 2 < npairs) else None
        fg_ = fg if k % 2 == 0 else None
        run_threads([[fg_, 1], [lg, 1], [ag, per(nt_acc, 18)], [bg, 1], [fg_, 1], [lg, 1], [tg, per(2 * k, 18)], [pg, 1]])
        if k == 1:
            for ex in range(4, 8):
                wcast(ex)
        if k == 2:
            for ex in range(8, N_EXP):
                wcast(ex)

    P.wait_all("sync", [P.lastw[k] for k in ("o0", "o1") if k in P.lastw])

    with nc.Block() as block:
        @block.sync
        def _(e):
            P.replay("sync", e)

        @block.scalar
        def _(e):
            P.replay("act", e)

        @block.vector
        def _(e):
            P.replay("dve", e)

        @block.gpsimd
        def _(e):
            P.replay("pool", e)

        @block.tensor
        def _(e):
            P.replay("pe", e)
    stack.close()
    return nc


def _prep_inputs(inp):
    f = np.float32
    w_in = np.asarray(inp["w_in"], f)[0]
    q = w_in[:, 0:512]; k = w_in[:, 512:640]; v = w_in[:, 640:768]; pool = w_in[:, 768:1280]
    qi = w_in[:, 1280:1792]; ki = w_in[:, 1792:1856]; wi = w_in[:, 1856:1864]
    cols = []
    for j in range(4):
        cols += [q[:, j * 64:(j + 1) * 64], q[:, (4 + j) * 64:(5 + j) * 64]]
    cols.append(k)
    cols.append(pool)
    for j in range(4):
        cols += [qi[:, j * 64:(j + 1) * 64], qi[:, (4 + j) * 64:(5 + j) * 64]]
    cols += [ki, ki, v, wi]
    w_in_p = np.ascontiguousarray(np.concatenate(cols, axis=1))
    assert w_in_p.shape == (D, NCOLS)
    lnp = np.ascontiguousarray(np.stack([np.asarray(inp[n], f)[0] for n in ("ln1_g", "ln1_b", "ln2_g", "ln2_b")]))
    w_router = np.ascontiguousarray(np.concatenate([np.asarray(inp["w_group_router"], f)[0],
                                                    np.asarray(inp["w_expert_router"], f)[0]], axis=1))
    b_router = np.ascontiguousarray(np.concatenate([np.asarray(inp["b_group_router"], f)[0],
                                                    np.asarray(inp["b_expert_router"], f)[0]]))
    wg = np.asarray(inp["w_gate"], f)[0].reshape(N_EXP, D, 256)
    wu = np.asarray(inp["w_up"], f)[0].reshape(N_EXP, D, 256)
    w_gu = np.ascontiguousarray(np.concatenate([wg, wu], axis=2))
    w_d = np.ascontiguousarray(np.asarray(inp["w_down"], f)[0].reshape(N_EXP, 256, D))
    shared = {
        "w_in_p": w_in_p,
        "w_pool": np.ascontiguousarray(np.asarray(inp["w_pool"], f)[0]),
        "pool_scale": np.ascontiguousarray(np.asarray(inp["pool_scale"], f)[0]),
        "w_out": np.ascontiguousarray(np.asarray(inp["w_out"], f)[0]),
        "ln_params": lnp, "w_router": w_router, "b_router": b_router, "w_gu": w_gu, "w_d": w_d,
    }
    x = np.asarray(inp["x"], f)
    maps = []
    for b in range(x.shape[0]):
        m = dict(shared)
        m["x"] = np.ascontiguousarray(x[b])
        m["xT"] = np.ascontiguousarray(x[b].T)
        maps.append(m)
    return maps


def kernel(**inputs):
    maps = _prep_inputs(inputs)
    nc = build_nc()
    res = run_bass_kernel_spmd(nc, maps, core_ids=list(range(8)))
    return np.stack([np.asarray(r["out"], np.float32) for r in res.results], axis=0)
```
